# Optimizing a Trainium2 kernel written in Bass

```python
import math
import jax, jax.numpy as jnp
from jax import lax
import numpy as np

D_MODEL = 2048
BATCH = 4
SEQ = 8192
DEPTH = 4

CHUNK = 64
N_META = 16
Q_BLOCK = 128
HA = 4
A_NOPE = 128
A_ROPE = 64
A_V = 128
Q_LORA = 512
KV_LORA = 256
ROPE_THETA = 10000.0
HB = 4
B_DH = 128
HC = 8
C_DH = 128
IDX_H = 8
IDX_DH = 64
TOPK_MAX = 256
N_BUCKETS = 32
MAX_DIST = 128
N_EXPERTS = 32
TOP_K = 4
D_FF = 512
SWIGLU_LIMIT = 7.0
SWIGLU_ALPHA = 1.702
MOE_BLOCK = 256
N_EVEN = (DEPTH + 1) // 2
N_ODD = DEPTH // 2
DN_ALPHA = (2 * DEPTH) ** 0.25
DN_BETA = (8 * DEPTH) ** -0.25
LN_EPS = 1e-5
RMS_EPS = 1e-6
NEG_INF = -1e30
EV_SPLITS = (Q_LORA, KV_LORA, A_ROPE, HB * B_DH, HB * B_DH, HB * B_DH)
EV_IN = sum(EV_SPLITS)
OD_SPLITS = (HC * C_DH, C_DH, C_DH, IDX_H * IDX_DH, IDX_DH, IDX_H)
OD_IN = sum(OD_SPLITS)
EV_OUT = HA * A_V + HB * B_DH
OD_OUT = HC * C_DH

kernel_name = "hybrid_mla_stickbreak_dsa_moe_trunk"


def layer_norm(x, g, b):
    xf = x.astype(jnp.float32)
    mu = jnp.mean(xf, axis=-1, keepdims=True)
    var = jnp.mean(jnp.square(xf - mu), axis=-1, keepdims=True)
    return ((xf - mu) * lax.rsqrt(var + LN_EPS) * g + b).astype(x.dtype)


def rms_norm(x, g):
    xf = x.astype(jnp.float32)
    return (xf * lax.rsqrt(jnp.mean(xf * xf, axis=-1, keepdims=True) + RMS_EPS) * g).astype(x.dtype)


def split_cols(h, sizes):
    return jnp.split(h, np.cumsum(sizes)[:-1].tolist(), axis=-1)


def chunk_ids(n):
    t = np.arange(n)
    return np.where(t < N_META, 0, (t - N_META) // CHUNK + 1)


def rope(x, pos):
    half = x.shape[-1] // 2
    inv = ROPE_THETA ** (-jnp.arange(half, dtype=jnp.float32) / half)
    ang = pos.astype(jnp.float32)[:, None] * inv[None, :]
    cos = jnp.cos(ang)[None, :, None, :]
    sin = jnp.sin(ang)[None, :, None, :]
    x1, x2 = x[..., :half], x[..., half:]
    return jnp.concatenate([x1 * cos - x2 * sin, x1 * sin + x2 * cos], axis=-1).astype(x.dtype)


def t5_bucket(rel):
    half = N_BUCKETS // 2
    max_exact = half // 2
    base = jnp.where(rel > 0, half, 0)
    n = jnp.abs(rel)
    nf = jnp.maximum(n, 1).astype(jnp.float32)
    large = max_exact + (jnp.log(nf / max_exact) / math.log(MAX_DIST / max_exact)
                         * (half - max_exact)).astype(jnp.int32)
    large = jnp.minimum(large, half - 1)
    return base + jnp.where(n < max_exact, n, large)


def sweep_query_blocks(fn, n, extend):
    outs = []
    for b in range(n // Q_BLOCK):
        start = b * Q_BLOCK
        outs.append(fn(start, min(n, start + Q_BLOCK + extend)))
    return jnp.concatenate(outs, axis=1)


def mla_attention(q, k, v, cid):
    n = q.shape[1]
    scale = (A_NOPE + A_ROPE) ** -0.5

    def block(start, kend):
        qb = q[:, start:start + Q_BLOCK]
        s = jnp.einsum('bqhd,bkhd->bhqk', qb, k[:, :kend]).astype(jnp.float32) * scale
        mask = cid[None, :kend] <= cid[start:start + Q_BLOCK, None]
        p = jax.nn.softmax(jnp.where(mask, s, NEG_INF), axis=-1)
        return jnp.einsum('bhqk,bkhd->bqhd', p.astype(v.dtype), v[:, :kend])

    return sweep_query_blocks(block, n, CHUNK)


def stick_breaking_attention(q, k, v):
    n = q.shape[1]
    scale = B_DH ** -0.5

    def block(start, kend):
        qb = q[:, start:start + Q_BLOCK]
        z = jnp.einsum('bqhd,bkhd->bhqk', qb, k[:, :kend]).astype(jnp.float32) * scale
        mask = np.arange(kend)[None, :] < (start + np.arange(Q_BLOCK))[:, None]
        log_keep = jnp.where(mask, jax.nn.log_sigmoid(-z), 0.0)
        cum = lax.cumsum(log_keep, axis=3, reverse=True)
        a = jnp.where(mask, jnp.exp(z + cum), 0.0)
        return jnp.einsum('bhqk,bkhd->bqhd', a.astype(v.dtype), v[:, :kend])

    return sweep_query_blocks(block, n, 0)


def indexer_sparse_attention(q, k, v, q_idx, k_idx, w_idx, cid, rel_table, k_sel):
    n = q.shape[1]
    cid_j = jnp.asarray(cid)

    def block(start, kend):
        qb = q[:, start:start + Q_BLOCK]
        qib = q_idx[:, start:start + Q_BLOCK]
        wb = w_idx[:, start:start + Q_BLOCK]
        cq = cid[start:start + Q_BLOCK]
        pq = start + np.arange(Q_BLOCK)
        rel_s = jax.nn.relu(jnp.einsum('bqhd,bkd->bqhk', qib, k_idx[:, :kend]).astype(jnp.float32)
                            * IDX_DH ** -0.5)
        score = jnp.einsum('bqh,bqhk->bqk', wb.astype(jnp.float32) * IDX_H ** -0.5, rel_s)
        admissible = cid[None, :kend] <= cq[:, None]
        score = jnp.where(admissible[None], score, NEG_INF)
        _, sel = lax.top_k(score, min(k_sel, kend))
        valid = cid_j[sel] <= cq[None, :, None]
        kg = jax.vmap(lambda kk, ii: kk[ii])(k, sel)
        vg = jax.vmap(lambda vv, ii: vv[ii])(v, sel)
        s = jnp.einsum('bqhd,bqkd->bqhk', qb, kg).astype(jnp.float32) * C_DH ** -0.5
        bias = rel_table[t5_bucket(sel - pq[None, :, None])].astype(jnp.float32)
        s = jnp.where(valid[:, :, None, :], s + jnp.swapaxes(bias, 2, 3), NEG_INF)
        p = jax.nn.softmax(s, axis=-1)
        return jnp.einsum('bqhk,bqkd->bqhd', p.astype(vg.dtype), vg)

    return sweep_query_blocks(block, n, CHUNK)


def even_mixer(h, pos, cid, w_in, q_norm, kv_norm, w_uq, w_ukv, w_out):
    bsz, n, _ = h.shape
    q_lat, kv_lat, k_rope, sb_q, sb_k, sb_v = split_cols(h @ w_in, EV_SPLITS)
    qa = (rms_norm(q_lat, q_norm) @ w_uq).reshape(bsz, n, HA, A_NOPE + A_ROPE)
    kva = (rms_norm(kv_lat, kv_norm) @ w_ukv).reshape(bsz, n, HA, A_NOPE + A_V)
    k_nope, v_a = kva[..., :A_NOPE], kva[..., A_NOPE:]
    k_r = jnp.broadcast_to(rope(k_rope[:, :, None, :], pos), (bsz, n, HA, A_ROPE))
    qa = jnp.concatenate([qa[..., :A_NOPE], rope(qa[..., A_NOPE:], pos)], axis=-1)
    ka = jnp.concatenate([k_nope, k_r], axis=-1)
    out_a = mla_attention(qa, ka, v_a, cid)
    shp = (bsz, n, HB, B_DH)
    out_b = stick_breaking_attention(sb_q.reshape(shp), sb_k.reshape(shp), sb_v.reshape(shp))
    merged = jnp.concatenate([out_a.reshape(bsz, n, HA * A_V), out_b.reshape(bsz, n, HB * B_DH)], axis=-1)
    return merged @ w_out


def odd_mixer(h, cid, w_in, w_out, rel_table, k_sel):
    bsz, n, _ = h.shape
    c_q, c_k, c_v, i_q, i_k, i_w = split_cols(h @ w_in, OD_SPLITS)
    out = indexer_sparse_attention(
        c_q.reshape(bsz, n, HC, C_DH), c_k, c_v,
        i_q.reshape(bsz, n, IDX_H, IDX_DH), i_k, i_w, cid, rel_table, k_sel)
    return out.reshape(bsz, n, HC * C_DH) @ w_out


def moe(h, router_w, router_b, w_in, b_in, w_out, b_out):
    bsz, n, d = h.shape
    n_tok = bsz * n
    t = h.reshape(n_tok, d)
    logits = (t @ router_w).astype(jnp.float32) + router_b
    top_val, top_idx = lax.top_k(logits, TOP_K)
    gates = jax.nn.softmax(top_val, axis=-1)
    n_assign = n_tok * TOP_K
    e_flat = top_idx.reshape(-1)
    order = jnp.argsort(e_flat)
    e_sorted = e_flat[order]
    counts = jnp.zeros((N_EXPERTS,), jnp.int32).at[e_flat].add(1)
    padded = (counts + MOE_BLOCK - 1) // MOE_BLOCK * MOE_BLOCK
    padded_end = jnp.cumsum(padded)
    start_sorted = jnp.cumsum(counts) - counts
    start_padded = padded_end - padded
    dest_sorted = start_padded[e_sorted] + jnp.arange(n_assign) - start_sorted[e_sorted]
    n_blocks = -(-n_assign // MOE_BLOCK) + N_EXPERTS
    row_tok = jnp.full((n_blocks * MOE_BLOCK,), n_tok, jnp.int32).at[dest_sorted].set(
        (order // TOP_K).astype(jnp.int32))
    block_e = jnp.minimum(jnp.searchsorted(padded_end, jnp.arange(n_blocks) * MOE_BLOCK, side='right'),
                          N_EXPERTS - 1)
    t_pad = jnp.concatenate([t, jnp.zeros((1, d), t.dtype)], axis=0)

    def run(args):
        rows, e = args
        hh = t_pad[rows] @ w_in[e] + b_in[e]
        gate, up = jnp.split(hh, 2, axis=-1)
        gate = jnp.minimum(gate, SWIGLU_LIMIT)
        up = jnp.clip(up, -SWIGLU_LIMIT, SWIGLU_LIMIT)
        return (gate * jax.nn.sigmoid(SWIGLU_ALPHA * gate) * (up + 1.0)) @ w_out[e] + b_out[e]

    y_rows = lax.map(run, (row_tok.reshape(n_blocks, MOE_BLOCK), block_e)).reshape(n_blocks * MOE_BLOCK, d)
    dest = jnp.zeros((n_assign,), jnp.int32).at[order].set(dest_sorted.astype(jnp.int32))
    y = y_rows[dest].reshape(n_tok, TOP_K, d)
    out = jnp.einsum('tk,tkd->td', gates.astype(y.dtype), y)
    return out.reshape(bsz, n, d)


def setup_inputs(seed: int = 0) -> dict:
    key = jax.random.key(seed)
    ks = jax.random.split(key, 20)
    f32 = jnp.float32
    D = D_MODEL

    def nrm(k, shape, scale):
        return jax.random.normal(k, shape, f32) * scale

    ev_col_scale = jnp.concatenate([jnp.ones((EV_IN - HB * B_DH,), f32), jnp.full((HB * B_DH,), DN_BETA, f32)])
    od_col_scale = jnp.concatenate([jnp.ones((HC * C_DH + C_DH,), f32),
                                    jnp.full((C_DH,), DN_BETA, f32),
                                    jnp.ones((OD_IN - HC * C_DH - 2 * C_DH,), f32)])
    ukv_col_scale = jnp.tile(jnp.concatenate([jnp.ones((A_NOPE,), f32), jnp.full((A_V,), DN_BETA, f32)]), HA)
    return {
        "x": nrm(ks[0], (BATCH, SEQ, D), 1.0),
        "meta_tokens": nrm(ks[1], (N_META, D), 1.0),
        "rel_bias_table": nrm(ks[2], (N_BUCKETS, HC), 0.2),
        "ev_w_in": nrm(ks[3], (N_EVEN, D, EV_IN), D ** -0.5) * ev_col_scale,
        "mla_q_norm": 1.0 + nrm(ks[4], (N_EVEN, Q_LORA), 0.02),
        "mla_kv_norm": 1.0 + nrm(ks[5], (N_EVEN, KV_LORA), 0.02),
        "mla_w_uq": nrm(ks[6], (N_EVEN, Q_LORA, HA * (A_NOPE + A_ROPE)), Q_LORA ** -0.5),
        "mla_w_ukv": nrm(ks[7], (N_EVEN, KV_LORA, HA * (A_NOPE + A_V)), KV_LORA ** -0.5) * ukv_col_scale,
        "ev_w_out": nrm(ks[8], (N_EVEN, EV_OUT, D), EV_OUT ** -0.5 * DN_BETA),
        "od_w_in": nrm(ks[9], (N_ODD, D, OD_IN), D ** -0.5) * od_col_scale,
        "od_w_out": nrm(ks[10], (N_ODD, OD_OUT, D), OD_OUT ** -0.5 * DN_BETA),
        "ln_g": 1.0 + nrm(ks[11], (DEPTH, 2, D), 0.02),
        "ln_b": nrm(ks[12], (DEPTH, 2, D), 0.02),
        "router_w": nrm(ks[13], (DEPTH, D, N_EXPERTS), D ** -0.5),
        "router_b": nrm(ks[14], (DEPTH, N_EXPERTS), 0.01),
        "exp_w_in": nrm(ks[15], (DEPTH, N_EXPERTS, D, 2 * D_FF), D ** -0.5),
        "exp_b_in": nrm(ks[16], (DEPTH, N_EXPERTS, 2 * D_FF), 0.01),
        "exp_w_out": nrm(ks[17], (DEPTH, N_EXPERTS, D_FF, D), D_FF ** -0.5 * DN_BETA),
        "exp_b_out": nrm(ks[18], (DEPTH, N_EXPERTS, D), 0.01),
    }


def reference(x, meta_tokens, rel_bias_table, ev_w_in, mla_q_norm, mla_kv_norm, mla_w_uq, mla_w_ukv,
              ev_w_out, od_w_in, od_w_out, ln_g, ln_b, router_w, router_b, exp_w_in, exp_b_in,
              exp_w_out, exp_b_out):
    bsz, seq, d = x.shape
    n_tot = seq + N_META
    n = -(-n_tot // Q_BLOCK) * Q_BLOCK
    h = jnp.concatenate([
        jnp.broadcast_to(meta_tokens[None].astype(x.dtype), (bsz, N_META, d)),
        x,
        jnp.zeros((bsz, n - n_tot, d), x.dtype)], axis=1)
    pos = jnp.arange(n)
    cid = chunk_ids(n)
    k_sel = min(TOPK_MAX, seq // 4)
    for i in range(DEPTH):
        j = i // 2
        if i % 2 == 0:
            m = even_mixer(h, pos, cid, ev_w_in[j], mla_q_norm[j], mla_kv_norm[j],
                           mla_w_uq[j], mla_w_ukv[j], ev_w_out[j])
        else:
            m = odd_mixer(h, cid, od_w_in[j], od_w_out[j], rel_bias_table, k_sel)
        h = layer_norm(DN_ALPHA * h + m, ln_g[i, 0], ln_b[i, 0])
        f = moe(h, router_w[i], router_b[i], exp_w_in[i], exp_b_in[i], exp_w_out[i], exp_b_out[i])
        h = layer_norm(DN_ALPHA * h + f, ln_g[i, 1], ln_b[i, 1])
    return h[:, N_META:N_META + seq]
```

```python
from contextlib import ExitStack, contextmanager
import numpy as np
import concourse.bass as bass
import concourse.mybir as mybir
from concourse.bass_utils import run_bass_kernel_spmd

F32 = mybir.dt.float32
BF16 = mybir.dt.bfloat16
I32 = mybir.dt.int32
ALU = mybir.AluOpType
AF = mybir.ActivationFunctionType
AX = mybir.AxisListType

D = 2048
N_META = 16
SEQ = 8192
NTOK = 8320
NT_ALL = 65
TPC = 33
DEPTH = 4
N_EXP = 32
D_FF = 512
CAP = 640
DN_ALPHA = (2 * DEPTH) ** 0.25
LN_EPS = 1e-5


class Buf:
    def __init__(self, name, t):
        self.name = name
        self.t = t
        self.sem = None
        self.dcnt = 0
        self.last_w = None
        self.readers = {}

    def ap(self):
        return self.t.ap()

    def __getitem__(self, k):
        return self.t.ap()[k]


class _Eng:
    def __init__(self, name, h, sem):
        self.name = name
        self.h = h
        self.sem = sem
        self.count = 0
        self.waited = {}


class Sched:
    SAME_ENGINE_SYNC = True

    def __init__(self, nc):
        self.nc = nc
        self.E = {}
        for name, h in (("pe", nc.tensor), ("act", nc.scalar), ("dve", nc.vector),
                        ("pool", nc.gpsimd), ("sp", nc.sync)):
            self.E[name] = _Eng(name, h, nc.alloc_semaphore(name="sem_" + name))
        self.bufs = []
        self._n = 0
        self.psum_banks = []
        self._pi = 0
        self._stack = None
        self._scope_bufs = None
        self._sem_pool = []

    def _reg(self, name, t):
        b = Buf(name, t)
        self.bufs.append(b)
        return b

    def sbuf(self, name, shape, dtype):
        self._n += 1
        nm = f"{name}_{self._n}"
        if self._stack is not None:
            b = self._reg(name, self._stack.enter_context(self.nc.sbuf_tensor(nm, list(shape), dtype)))
            self._scope_bufs.append(b)
            return b
        return self._reg(name, self.nc.alloc_sbuf_tensor(nm, list(shape), dtype))

    @contextmanager
    def scope(self):
        st = ExitStack()
        prev, self._stack = self._stack, st
        prevb, self._scope_bufs = self._scope_bufs, []
        try:
            yield
        finally:
            self.barrier()
            for b in self._scope_bufs:
                if b.sem is not None:
                    self._sem_pool.append((b.sem, b.dcnt))
                    b.sem = None
                self.bufs.remove(b)
            self._stack = prev
            self._scope_bufs = prevb
            st.close()

    def dram(self, name, shape, dtype, kind="Internal"):
        return self._reg(name, self.nc.dram_tensor(name, list(shape), dtype, kind=kind))

    def init_psum(self, n=8):
        for i in range(n):
            self.psum_banks.append(self._reg(f"ps{i}", self.nc.alloc_psum_tensor(f"ps{i}", [128, 512], F32)))

    def psum(self):
        b = self.psum_banks[self._pi % len(self.psum_banks)]
        self._pi += 1
        return b

    def psum_hold(self):
        b = self.psum()
        self.psum_banks.remove(b)
        return b

    def psum_release(self, b):
        self.psum_banks.append(b)

    def _wait(self, e, deps):
        for sem, val in deps.items():
            if sem is e.sem and (e.name == "pe" or not self.SAME_ENGINE_SYNC):
                continue
            if e.waited.get(sem, 0) < val:
                e.h.wait_ge(sem, val)
                e.waited[sem] = val

    @staticmethod
    def _add(deps, ev):
        if ev is not None:
            sem, val = ev
            if deps.get(sem, 0) < val:
                deps[sem] = val

    def op(self, eng, fn, reads=(), writes=()):
        e = self.E[eng]
        deps = {}
        for b in reads:
            self._add(deps, b.last_w)
        for b in writes:
            self._add(deps, b.last_w)
            for sem, val in b.readers.items():
                self._add(deps, (sem, val))
        self._wait(e, deps)
        ins = fn(e.h)
        e.count += 1
        ins.then_inc(e.sem, 1)
        ev = (e.sem, e.count)
        for b in writes:
            b.last_w = ev
            b.readers = {}
        for b in reads:
            if b not in writes:
                b.readers[e.sem] = e.count
        return ins

    def dma(self, q, out_buf, out_ap, in_buf, in_ap, indirect=None, **kw):
        e = self.E[q]
        deps = {}
        self._add(deps, in_buf.last_w)
        if out_buf.last_w is not None and out_buf.last_w[0] is not out_buf.sem:
            self._add(deps, out_buf.last_w)
        for sem, val in out_buf.readers.items():
            self._add(deps, (sem, val))
        extra = kw.pop("extra_reads", ())
        for b in extra:
            self._add(deps, b.last_w)
        self._wait(e, deps)
        if out_buf.sem is None:
            if self._sem_pool:
                out_buf.sem, out_buf.dcnt = self._sem_pool.pop()
            else:
                self._n += 1
                out_buf.sem = self.nc.alloc_semaphore(name=f"dsem_{out_buf.name}_{self._n}")
        if indirect is None:
            ins = e.h.dma_start(out=out_ap, in_=in_ap, **kw)
        else:
            ins = e.h.indirect_dma_start(out=out_ap, in_=in_ap, **indirect, **kw)
        out_buf.dcnt += 1
        ins.then_inc(out_buf.sem, 16)
        ev = (out_buf.sem, 16 * out_buf.dcnt)
        out_buf.last_w = ev
        out_buf.readers = {}
        in_buf.readers[out_buf.sem] = 16 * out_buf.dcnt
        for b in extra:
            b.readers[out_buf.sem] = 16 * out_buf.dcnt
        return ins

    def barrier(self, engines=None):
        names = engines or list(self.E)
        for n in names:
            e = self.E[n]
            deps = {}
            for o in self.E.values():
                if o is not e and o.count:
                    deps[o.sem] = o.count
            for b in self.bufs:
                if b.sem is not None and b.dcnt:
                    deps[b.sem] = 16 * b.dcnt
            self._wait(e, deps)

    def mm(self, ps, out_ap, lb, lhsT, rb, rhs, start, stop):
        return self.op("pe", lambda h: h.matmul(out_ap, lhsT, rhs, start=start, stop=stop),
                       reads=(lb, rb), writes=(ps,))

    def tr(self, ps, out_ap, ib, in_ap, idb, id_ap):
        return self.op("pe", lambda h: h.transpose(out_ap, in_ap, id_ap), reads=(ib, idb), writes=(ps,))


def _ln_tile(S, z, zb, gbc, bbc, out, st, tmp):
    nc = S.nc
    nch = D // 512
    stats = st[:, 0:nch * 6].rearrange("p (c s) -> p c s", s=6)
    for c in range(nch):
        S.op("dve", lambda h, c=c: h.bn_stats(out=stats[:, c, :], in_=z[:, c * 512:(c + 1) * 512]),
             reads=(zb,), writes=(tmp,))
    mv = st[:, 32:34]
    S.op("dve", lambda h: h.bn_aggr(out=mv, in_=stats), reads=(tmp,), writes=(tmp,))
    rstd = st[:, 34:35]
    nmr = st[:, 35:36]
    S.op("dve", lambda h: h.tensor_scalar(out=rstd, in0=mv[:, 1:2], scalar1=LN_EPS, scalar2=None,
                                          op0=ALU.add), reads=(tmp,), writes=(tmp,))
    S.op("act", lambda h: h.sqrt(out=rstd, in_=rstd), reads=(tmp,), writes=(tmp,))
    S.op("dve", lambda h: h.reciprocal(out=rstd, in_=rstd), reads=(tmp,), writes=(tmp,))
    S.op("dve", lambda h: h.scalar_tensor_tensor(out=nmr, in0=mv[:, 0:1], scalar=-1.0, in1=rstd,
                                                 op0=ALU.mult, op1=ALU.mult), reads=(tmp,), writes=(tmp,))
    return rstd, nmr


def emit_moe(S, io, T):
    nc = S.nc
    NS = CAP // 128
    h, out = io["h"], io["out"]
    xbuf = S.dram("xbuf", [N_EXP * CAP, D], BF16)
    ybuf = S.dram("ybuf", [N_EXP * CAP, D], BF16)

    cst = S.sbuf("cst", [128, 416], F32)
    S.dma("sp", cst, cst.ap(), io["cst"], io["cst"].ap())
    ident = cst[:, 0:128]
    identb = S.sbuf("identb", [128, 128], BF16)
    trib = S.sbuf("trib", [128, 128], BF16)
    onesb = S.sbuf("onesb", [128, 128], BF16)
    S.op("dve", lambda h_: h_.tensor_copy(out=identb.ap(), in_=cst[:, 0:128]), reads=(cst,), writes=(identb,))
    S.op("dve", lambda h_: h_.tensor_copy(out=trib.ap(), in_=cst[:, 128:256]), reads=(cst,), writes=(trib,))
    S.op("dve", lambda h_: h_.tensor_copy(out=onesb.ap(), in_=cst[:, 256:384]), reads=(cst,), writes=(onesb,))
    ecap = cst[:, 384:416]
    rw = S.sbuf("rw", [128, 16, 32], F32)
    S.dma("sp", rw, rw.ap(), io["rw"], io["rw"].ap().rearrange("(c p) e -> p c e", p=128))
    rbb = S.sbuf("rbb", [128, 32], F32)
    S.dma("sp", rbb, rbb.ap(), io["rb"], io["rb"].ap().partition_broadcast(128))
    gbc = S.sbuf("gbc", [128, D], F32)
    bbc = S.sbuf("bbc", [128, D], F32)
    S.dma("sp", gbc, gbc.ap(), io["lng"], io["lng"].ap().partition_broadcast(128))
    S.dma("sp", bbc, bbc.ap(), io["lnb"], io["lnb"].ap().partition_broadcast(128))
    b1s = S.sbuf("b1s", [32, 1024], F32)
    S.dma("sp", b1s, b1s.ap(), io["b1"], io["b1"].ap())
    b1T = S.sbuf("b1T", [128, 8, 32], F32)
    for c in range(8):
        ps = S.psum()
        S.tr(ps, ps[:, 0:32], b1s, b1s[:, c * 128:(c + 1) * 128], cst, cst[0:32, 0:32])
        S.op("dve", lambda h_, c=c, ps=ps: h_.tensor_copy(out=b1T[:, c, :], in_=ps[:, 0:32]),
             reads=(ps,), writes=(b1T,))
    bc_reg = nc.gpsimd.alloc_register("bc_reg")
    nc.gpsimd.reg_mov(bc_reg, N_EXP * CAP - 1)
    dest_f = S.sbuf("dest_f", [128, T * 4], F32)
    dest_i = S.sbuf("dest_i", [128, T * 4], I32)
    gate_all = S.sbuf("gate_all", [128, T * 4], F32)
    tot = S.sbuf("tot", [128, 32], F32)
    S.op("dve", lambda h_: h_.memset(tot.ap(), 0.0), writes=(tot,))

    ph = S.scope()
    ph.__enter__()
    hs = [S.sbuf(f"hs{i}", [128, D], F32) for i in range(2)]
    hb = [S.sbuf(f"hb{i}", [128, D], BF16) for i in range(2)]
    hT = S.sbuf("hT", [128, D], F32)
    sm = S.sbuf("sm", [128, 512], F32)
    LG, M8, NMX, EX, EXM, SS, RS, G, SLOT, OH, TMP = (
        sm[:, 0:32], sm[:, 32:40], sm[:, 40:41], sm[:, 48:80], sm[:, 80:112], sm[:, 112:113],
        sm[:, 113:114], sm[:, 128:160], sm[:, 160:192], sm[:, 192:224], sm[:, 224:256])
    maskb = S.sbuf("maskb", [128, 32], BF16)
    for i in range(T):
        x = hs[i % 2]
        S.dma("sp", x, x.ap(), h, h[i * 128:(i + 1) * 128, :])
        xb = hb[i % 2]
        S.op("act", lambda h_, x=x, xb=xb: h_.copy(out=xb.ap(), in_=x.ap()), reads=(x,), writes=(xb,))
        for g4 in range(4):
            ps = S.psum()
            for j in range(4):
                c = g4 * 4 + j
                S.tr(ps, ps[:, j * 128:(j + 1) * 128], x, x[:, c * 128:(c + 1) * 128], cst, ident)
            S.op("dve" if g4 % 2 else "act",
                 (lambda h_, ps=ps, g4=g4: h_.tensor_copy(out=hT[:, g4 * 512:(g4 + 1) * 512], in_=ps.ap())) if g4 % 2 else
                 (lambda h_, ps=ps, g4=g4: h_.copy(out=hT[:, g4 * 512:(g4 + 1) * 512], in_=ps.ap())),
                 reads=(ps,), writes=(hT,))
        ps = S.psum()
        for c in range(16):
            S.mm(ps, ps[:, 0:32], hT, hT[:, c * 128:(c + 1) * 128], rw, rw[:, c, :], c == 0, c == 15)
        S.op("dve", lambda h_, ps=ps: h_.tensor_tensor(out=LG, in0=ps[:, 0:32], in1=rbb.ap(), op=ALU.add),
             reads=(ps, rbb), writes=(sm,))
        S.op("dve", lambda h_: h_.max(out=M8, in_=LG), reads=(sm,), writes=(sm,))
        S.op("dve", lambda h_: h_.tensor_scalar(out=maskb.ap(), in0=LG, scalar1=M8[:, 3:4], scalar2=None,
                                                op0=ALU.is_ge), reads=(sm,), writes=(maskb,))
        S.op("dve", lambda h_: h_.tensor_scalar(out=NMX, in0=M8[:, 0:1], scalar1=-1.0, scalar2=None,
                                                op0=ALU.mult), reads=(sm,), writes=(sm,))
        S.op("act", lambda h_: h_.activation(out=EX, in_=LG, func=AF.Exp, bias=NMX, scale=1.0),
             reads=(sm,), writes=(sm,))
        S.op("dve", lambda h_: h_.tensor_tensor(out=EXM, in0=EX, in1=maskb.ap(), op=ALU.mult),
             reads=(sm, maskb), writes=(sm,))
        S.op("dve", lambda h_: h_.reduce_sum(out=SS, in_=EXM, axis=AX.X), reads=(sm,), writes=(sm,))
        S.op("dve", lambda h_: h_.reciprocal(out=RS, in_=SS), reads=(sm,), writes=(sm,))
        S.op("dve", lambda h_: h_.tensor_scalar(out=G, in0=EXM, scalar1=RS, scalar2=None, op0=ALU.mult),
             reads=(sm,), writes=(sm,))
        ps2 = S.psum()
        S.mm(ps2, ps2[:, 0:32], trib, trib.ap(), maskb, maskb.ap(), True, True)
        S.mm(ps2, ps2[:, 32:64], onesb, onesb.ap(), maskb, maskb.ap(), True, True)
        S.op("dve", lambda h_, ps2=ps2: h_.tensor_tensor(out=SLOT, in0=ps2[:, 0:32], in1=tot.ap(), op=ALU.add),
             reads=(ps2, tot), writes=(sm,))
        S.op("dve", lambda h_, ps2=ps2: h_.tensor_tensor(out=tot.ap(), in0=ps2[:, 32:64], in1=tot.ap(), op=ALU.add),
             reads=(ps2, tot), writes=(tot,))
        S.op("dve", lambda h_: h_.tensor_scalar(out=TMP, in0=SLOT, scalar1=float(CAP), scalar2=1.0e6,
                                                op0=ALU.is_ge, op1=ALU.mult), reads=(sm,), writes=(sm,))
        S.op("dve", lambda h_: h_.tensor_tensor(out=SLOT, in0=SLOT, in1=TMP, op=ALU.add), reads=(sm,), writes=(sm,))
        S.op("dve", lambda h_: h_.tensor_tensor(out=SLOT, in0=SLOT, in1=ecap, op=ALU.add),
             reads=(sm, cst), writes=(sm,))
        for k in range(4):
            col = i * 4 + k
            S.op("dve", lambda h_, k=k: h_.tensor_scalar(out=OH, in0=LG, scalar1=M8[:, k:k + 1], scalar2=None,
                                                         op0=ALU.is_equal), reads=(sm,), writes=(sm,))
            S.op("dve", lambda h_: h_.tensor_tensor(out=TMP, in0=OH, in1=SLOT, op=ALU.mult), reads=(sm,), writes=(sm,))
            S.op("dve", lambda h_, col=col: h_.reduce_sum(out=dest_f[:, col:col + 1], in_=TMP, axis=AX.X),
                 reads=(sm,), writes=(dest_f,))
            S.op("dve", lambda h_: h_.tensor_tensor(out=TMP, in0=OH, in1=G, op=ALU.mult), reads=(sm,), writes=(sm,))
            S.op("dve", lambda h_, col=col: h_.reduce_sum(out=gate_all[:, col:col + 1], in_=TMP, axis=AX.X),
                 reads=(sm,), writes=(gate_all,))
        S.op("dve", lambda h_, i=i: h_.tensor_copy(out=dest_i[:, i * 4:(i + 1) * 4], in_=dest_f[:, i * 4:(i + 1) * 4]),
             reads=(dest_f,), writes=(dest_i,))
        for k in range(4):
            col = i * 4 + k
            S.dma("pool", xbuf, xbuf.ap(), xb, xb.ap(), extra_reads=(dest_i,),
                  indirect=dict(out_offset=bass.IndirectOffsetOnAxis(ap=dest_i[:, col:col + 1], axis=0),
                                in_offset=None, bounds_check=bc_reg, oob_is_err=False))
    S.op("dve", lambda h_: h_.tensor_scalar(out=dest_f.ap(), in0=dest_f.ap(), scalar1=float(N_EXP * CAP), scalar2=None,
                                            op0=ALU.is_lt), reads=(dest_f,), writes=(dest_f,))
    S.op("dve", lambda h_: h_.tensor_tensor(out=gate_all.ap(), in0=gate_all.ap(), in1=dest_f.ap(), op=ALU.mult),
         reads=(dest_f, gate_all), writes=(gate_all,))

    ph.__exit__(None, None, None)
    ph = S.scope()
    ph.__enter__()
    w1s = [S.sbuf(f"w1s{i}", [128, 16, 1024], BF16) for i in range(2)]
    w2s = [S.sbuf(f"w2s{i}", [128, 4, D], BF16) for i in range(2)]
    b2r = [S.sbuf(f"b2r{i}", [1, D], BF16) for i in range(2)]
    xs = [S.sbuf(f"xs{i}", [128, D], BF16) for i in range(2)]
    XT = [S.sbuf(f"XT{i}", [128, 16, CAP], BF16) for i in range(1)]
    AT = [S.sbuf(f"AT{i}", [128, 4, CAP], BF16) for i in range(2)]
    gg = [S.sbuf(f"gg{i}", [128, CAP], F32) for i in range(2)]
    sg = [S.sbuf(f"sg{i}", [128, CAP], F32) for i in range(2)]
    uu = [S.sbuf(f"uu{i}", [128, CAP], F32) for i in range(2)]
    ys = [S.sbuf(f"ys{i}", [128, D], BF16) for i in range(2)]
    w1d, w2d, b2d = io["w1"], io["w2"], io["b2"]

    def load_w(e):
        p = e % 2
        for q in range(4):
            S.dma("pool", w1s[p], w1s[p][:, q * 4:(q + 1) * 4, :], w1d,
                  w1d.ap()[e, q * 512:(q + 1) * 512, :].rearrange("(c p) f -> p c f", p=128))
        for q in range(2):
            S.dma("pool", w2s[p], w2s[p][:, q * 2:(q + 1) * 2, :], w2d,
                  w2d.ap()[e, q * 256:(q + 1) * 256, :].rearrange("(c p) f -> p c f", p=128))
        S.dma("pool", b2r[p], b2r[p].ap(), b2d, b2d.ap()[e:e + 1, :])

    load_w(0)
    nx = 0
    for e in range(N_EXP):
        p = e % 2
        if e + 1 < N_EXP:
            load_w(e + 1)
        xt = XT[0]
        for s in range(NS):
            x = xs[nx % 2]
            nx += 1
            S.dma("sp", x, x.ap(), xbuf, xbuf[e * CAP + s * 128: e * CAP + (s + 1) * 128, :])
            for g2 in range(2):
                ps = S.psum()
                psb = ps.ap().bitcast(BF16)
                for j in range(8):
                    c = g2 * 8 + j
                    S.tr(ps, psb[:, j * 128:(j + 1) * 128], x, x[:, c * 128:(c + 1) * 128], identb, identb.ap())
                src = psb.rearrange("p (c t) -> p c t", t=128)
                dst = xt[:, g2 * 8:(g2 + 1) * 8, s * 128:(s + 1) * 128]
                if g2 == 0:
                    S.op("dve", lambda h_, src=src, dst=dst: h_.tensor_copy(out=dst, in_=src), reads=(ps,), writes=(xt,))
                else:
                    S.op("act", lambda h_, src=src, dst=dst: h_.copy(out=dst, in_=src), reads=(ps,), writes=(xt,))
        at = AT[p]
        for fc in range(4):
            pg0, pg1, pu0, pu1 = S.psum(), S.psum(), S.psum(), S.psum()
            for (pa, pb_, f) in ((pg0, pg1, fc), (pu0, pu1, fc + 4)):
                for c in range(16):
                    S.mm(pa, pa[:, 0:512], w1s[p], w1s[p][:, c, f * 128:(f + 1) * 128], xt, xt[:, c, 0:512], c == 0, c == 15)
                for c in range(16):
                    S.mm(pb_, pb_[:, 0:CAP - 512], w1s[p], w1s[p][:, c, f * 128:(f + 1) * 128], xt, xt[:, c, 512:CAP],
                         c == 0, c == 15)
            g_, s_, u_ = gg[fc % 2], sg[fc % 2], uu[fc % 2]
            for (pa, lo, hi) in ((pg0, 0, 512), (pg1, 512, CAP)):
                S.op("dve", lambda h_, pa=pa, lo=lo, hi=hi, g_=g_, fc=fc: h_.tensor_scalar(
                    out=g_[:, lo:hi], in0=pa[:, 0:hi - lo], scalar1=b1T[:, fc, e:e + 1], scalar2=7.0,
                    op0=ALU.add, op1=ALU.min), reads=(pa, b1T), writes=(g_,))
            for (pa, lo, hi) in ((pu0, 0, 512), (pu1, 512, CAP)):
                S.op("dve", lambda h_, pa=pa, lo=lo, hi=hi, u_=u_, fc=fc: h_.tensor_scalar(
                    out=u_[:, lo:hi], in0=pa[:, 0:hi - lo], scalar1=b1T[:, fc + 4, e:e + 1], scalar2=7.0,
                    op0=ALU.add, op1=ALU.min), reads=(pa, b1T), writes=(u_,))
            S.op("act", lambda h_, g_=g_, s_=s_: h_.activation(out=s_.ap(), in_=g_.ap(), func=AF.Sigmoid, scale=1.702),
                 reads=(g_,), writes=(s_,))
            S.op("pool", lambda h_, u_=u_: h_.tensor_scalar(out=u_.ap(), in0=u_.ap(), scalar1=-7.0, scalar2=1.0,
                                                             op0=ALU.max, op1=ALU.add), reads=(u_,), writes=(u_,))
            S.op("pool", lambda h_, g_=g_, s_=s_: h_.tensor_tensor(out=s_.ap(), in0=s_.ap(), in1=g_.ap(), op=ALU.mult),
                 reads=(g_, s_), writes=(s_,))
            S.op("pool", lambda h_, u_=u_, s_=s_, at=at, fc=fc: h_.tensor_tensor(out=at[:, fc, :], in0=s_.ap(), in1=u_.ap(),
                                                                                 op=ALU.mult),
                 reads=(u_, s_), writes=(at,))
        for s in range(NS):
            y = ys[s % 2]
            for cg in range(4):
                ps = S.psum()
                for fc in range(4):
                    S.mm(ps, ps.ap(), at, at[:, fc, s * 128:(s + 1) * 128], w2s[p], w2s[p][:, fc, cg * 512:(cg + 1) * 512],
                         fc == 0, False)
                S.mm(ps, ps.ap(), onesb, onesb[0:1, :], b2r[p], b2r[p][0:1, cg * 512:(cg + 1) * 512], False, True)
                if cg % 2 == 0:
                    S.op("dve", lambda h_, ps=ps, y=y, cg=cg: h_.tensor_copy(out=y[:, cg * 512:(cg + 1) * 512], in_=ps.ap()),
                         reads=(ps,), writes=(y,))
                else:
                    S.op("act", lambda h_, ps=ps, y=y, cg=cg: h_.copy(out=y[:, cg * 512:(cg + 1) * 512], in_=ps.ap()),
                         reads=(ps,), writes=(y,))
            S.dma("sp", ybuf, ybuf[e * CAP + s * 128: e * CAP + (s + 1) * 128, :], y, y.ap())

    ph.__exit__(None, None, None)
    ph = S.scope()
    ph.__enter__()
    hs = [S.sbuf(f"hs{i}", [128, D], F32) for i in range(2)]
    yg = [S.sbuf(f"yg{i}", [128, D], BF16) for i in range(8)]
    for b in yg:
        S.op("pool", lambda h_, b=b: h_.memset(b.ap(), 0.0), writes=(b,))
    zz = [S.sbuf(f"zz{i}", [128, D], F32) for i in range(2)]
    oo = [S.sbuf(f"oo{i}", [128, D], F32) for i in range(2)]
    st = S.sbuf("lnst", [128, 64], F32)
    for i in range(T):
        x = hs[i % 2]
        S.dma("sp", x, x.ap(), h, h[i * 128:(i + 1) * 128, :])
        ygs = yg[(i % 2) * 4:(i % 2) * 4 + 4]
        for k in range(4):
            col = i * 4 + k
            S.dma("pool", ygs[k], ygs[k].ap(), ybuf, ybuf.ap(), extra_reads=(dest_i,),
                  indirect=dict(out_offset=None,
                                in_offset=bass.IndirectOffsetOnAxis(ap=dest_i[:, col:col + 1], axis=0),
                                bounds_check=bc_reg, oob_is_err=False))
        z = zz[i % 2]
        S.op("act", lambda h_, z=z, x=x: h_.mul(out=z.ap(), in_=x.ap(), mul=float(DN_ALPHA)), reads=(x,), writes=(z,))
        for k in range(4):
            col = i * 4 + k
            S.op("dve", lambda h_, z=z, k=k, col=col, ygs=ygs: h_.scalar_tensor_tensor(
                out=z.ap(), in0=ygs[k].ap(), scalar=gate_all[:, col:col + 1], in1=z.ap(), op0=ALU.mult, op1=ALU.add),
                reads=(ygs[k], gate_all, z), writes=(z,))
        rstd, nmr = _ln_tile(S, z, z, gbc, bbc, None, st, st)
        o = oo[i % 2]
        S.op("act", lambda h_, z=z, o=o: h_.activation(out=o.ap(), in_=z.ap(), func=AF.Identity, bias=nmr, scale=rstd),
             reads=(z, st), writes=(o,))
        S.op("pool", lambda h_, o=o: h_.tensor_tensor(out=o.ap(), in0=o.ap(), in1=gbc.ap(), op=ALU.mult),
             reads=(o, gbc), writes=(o,))
        S.op("pool", lambda h_, o=o: h_.tensor_tensor(out=o.ap(), in0=o.ap(), in1=bbc.ap(), op=ALU.add),
             reads=(o, bbc), writes=(o,))
        S.dma("sp", out, out[i * 128:(i + 1) * 128, :], o, o.ap())
    ph.__exit__(None, None, None)


def moe_consts():
    c = np.zeros((128, 416), np.float32)
    c[:, 0:128] = np.eye(128, dtype=np.float32)
    c[:, 128:256] = np.triu(np.ones((128, 128), np.float32), 1)
    c[:, 256:384] = 1.0
    c[:, 384:416] = (np.arange(N_EXP, dtype=np.float32) * CAP)[None, :]
    return c


def build_moe(T):
    nc = bass.Bass("TRN2", target_bir_lowering=False)
    S = Sched(nc)
    S.init_psum()
    io = {}
    for name, shape in (("h", [T * 128, D]), ("rw", [D, N_EXP]), ("rb", [1, N_EXP]), ("w1", [N_EXP, D, 2 * D_FF]),
                        ("b1", [N_EXP, 2 * D_FF]), ("w2", [N_EXP, D_FF, D]), ("b2", [N_EXP, D]),
                        ("lng", [1, D]), ("lnb", [1, D]), ("cst", [128, 416])):
        io[name] = S.dram(name, shape, F32, kind="ExternalInput")
    io["out"] = S.dram("out", [T * 128, D], F32, kind="ExternalOutput")
    emit_moe(S, io, T)
    S.barrier(["sp"])
    return nc


Q_LORA, KV_LORA, A_ROPE, A_NOPE, A_V, HA, HB, B_DH = 512, 256, 64, 128, 128, 4, 4, 128
EV_IN = 2368
RMS_EPS = 1e-6


def load_cast(S, dst, dst_ap, src, src_ap):
    return S.dma("pool", dst, dst_ap, src, src_ap)


def emit_transpose_h(S, x, hT, t4, ident_b, ident_ap):
    for g4 in range(4):
        ps = S.psum()
        for j in range(4):
            c = g4 * 4 + j
            S.tr(ps, ps[:, j * 128:(j + 1) * 128], x, x[:, c * 128:(c + 1) * 128], ident_b, ident_ap)
        src = ps.ap().rearrange("p (c t) -> p c t", t=128)
        dst = hT[:, g4 * 4:(g4 + 1) * 4, t4 * 128:(t4 + 1) * 128]
        if g4 % 2 == 0:
            S.op("dve", lambda h_, src=src, dst=dst: h_.tensor_copy(out=dst, in_=src), reads=(ps,), writes=(hT,))
        else:
            S.op("act", lambda h_, src=src, dst=dst: h_.copy(out=dst, in_=src), reads=(ps,), writes=(hT,))


def emit_ev_a(S, io, T):
    nc = S.nc
    NTK = T * 128
    ph = S.scope()
    ph.__enter__()
    cst = S.sbuf("cstA", [128, 128], F32)
    S.dma("sp", cst, cst.ap(), io["cst"], io["cst"].ap()[:, 0:128])
    identb = S.sbuf("identbA", [128, 128], BF16)
    S.op("dve", lambda h_: h_.tensor_copy(out=identb.ap(), in_=cst.ap()), reads=(cst,), writes=(identb,))
    win = S.sbuf("win", [128, 16, EV_IN], BF16)
    wd = io["w_in"]
    for c in range(16):
        load_cast(S, win, win[:, c, :], wd, wd.ap()[c * 128:(c + 1) * 128, :])
    winsw = S.sbuf("winsw", [128, 16, 64], BF16)
    wv = wd.ap().rearrange("(c p) f -> p c f", p=128)
    load_cast(S, winsw, winsw[:, :, 0:32], wd, wv[:, :, 800:832])
    load_cast(S, winsw, winsw[:, :, 32:64], wd, wv[:, :, 768:800])
    wuq = S.sbuf("wuq", [128, 4, 768], BF16)
    wuqf = S.sbuf("wuqf", [128, 4, 768], F32)
    S.dma("sp", wuqf, wuqf.ap(), io["w_uq"], io["w_uq"].ap().rearrange("(c p) f -> p c f", p=128))
    wukv = S.sbuf("wukv", [128, 2, 1024], BF16)
    wukvf = S.sbuf("wukvf", [128, 2, 1024], F32)
    S.dma("sp", wukvf, wukvf.ap(), io["w_ukv"], io["w_ukv"].ap().rearrange("(c p) f -> p c f", p=128))
    gq = S.sbuf("gq", [128, 4], F32)
    gkv = S.sbuf("gkv", [128, 2], F32)
    with nc.allow_non_contiguous_dma(reason="tiny gain vectors"):
        S.dma("sp", gq, gq.ap(), io["qn"], io["qn"].ap().rearrange("o (c p) -> p (o c)", p=128))
        S.dma("sp", gkv, gkv.ap(), io["kvn"], io["kvn"].ap().rearrange("o (c p) -> p (o c)", p=128))
    for c in range(4):
        S.op("dve", lambda h_, c=c: h_.tensor_scalar(out=wuq[:, c, :], in0=wuqf[:, c, :], scalar1=gq[:, c:c + 1],
                                                     scalar2=None, op0=ALU.mult), reads=(wuqf, gq), writes=(wuq,))
    for c in range(2):
        S.op("dve", lambda h_, c=c: h_.tensor_scalar(out=wukv[:, c, :], in0=wukvf[:, c, :], scalar1=gkv[:, c:c + 1],
                                                     scalar2=None, op0=ALU.mult), reads=(wukvf, gkv), writes=(wukv,))
    wuqsw = S.sbuf("wuqsw", [128, 4, 256], BF16)
    wuv = wuq.ap().rearrange("p c (h x) -> p c h x", x=192)
    swv = wuqsw.ap().rearrange("p c (h x) -> p c h x", x=64)
    for c in range(4):
        S.op("dve", lambda h_, c=c: h_.tensor_copy(out=swv[:, c, :, 0:32], in_=wuv[:, c, :, 160:192]),
             reads=(wuq,), writes=(wuqsw,))
        S.op("dve", lambda h_, c=c: h_.tensor_copy(out=swv[:, c, :, 32:64], in_=wuv[:, c, :, 128:160]),
             reads=(wuq,), writes=(wuqsw,))
    wv_v = S.sbuf("wv_v", [128, 2, 512], BF16)
    kvv = wukv.ap().rearrange("p c (h x) -> p c h x", x=256)
    for c in range(2):
        S.op("dve", lambda h_, c=c: h_.tensor_copy(out=wv_v[:, c, :].rearrange("p (h x) -> p h x", x=128),
                                                   in_=kvv[:, c, :, 128:256]), reads=(wukv,), writes=(wv_v,))

    hs = [S.sbuf(f"hsA{i}", [128, D], F32) for i in range(2)]
    hT = S.sbuf("hTA", [128, 16, 512], BF16)
    lat = S.sbuf("lat", [128, 768], F32)
    latn = S.sbuf("latn", [128, 768], BF16)
    junk = S.sbuf("junkA", [128, 512], F32)
    rr = S.sbuf("rrA", [128, 8], F32)
    latnT = S.sbuf("latnT", [128, 6, 512], BF16)
    ctab = S.sbuf("ctab", [64, 512], F32)
    stab = S.sbuf("stab", [64, 512], F32)
    fo = [S.sbuf(f"foA{i}", [128, 512], BF16) for i in range(3)]
    to = [S.sbuf(f"toA{i}", [128, 512], BF16) for i in range(2)]
    rtmp = S.sbuf("rtmpA", [64, 512], F32)
    nfo = [0]
    nto = [0]

    def fm_out(ps, rows, n, dram, dap):
        f = fo[nfo[0] % 3]
        nfo[0] += 1
        if nfo[0] % 2:
            S.op("dve", lambda h_: h_.tensor_copy(out=f[0:rows, 0:n], in_=ps[0:rows, 0:n]), reads=(ps,), writes=(f,))
        else:
            S.op("act", lambda h_: h_.copy(out=f[0:rows, 0:n], in_=ps[0:rows, 0:n]), reads=(ps,), writes=(f,))
        S.dma("sp", dram, dap, f, f[0:rows, 0:n])

    def rope_out(psa, psb, n, dram, dap):
        f = fo[nfo[0] % 3]
        nfo[0] += 1
        S.op("dve", lambda h_: h_.tensor_tensor(out=rtmp[:, 0:n], in0=psa[0:64, 0:n], in1=ctab[:, 0:n], op=ALU.mult),
             reads=(psa, ctab), writes=(rtmp,))
        S.op("dve", lambda h_: h_.tensor_tensor(out=junk[0:64, 0:n], in0=psb[0:64, 0:n], in1=stab[:, 0:n], op=ALU.mult),
             reads=(psb, stab), writes=(junk,))
        S.op("dve", lambda h_: h_.tensor_tensor(out=f[0:64, 0:n], in0=rtmp[:, 0:n], in1=junk[0:64, 0:n], op=ALU.add),
             reads=(rtmp, junk), writes=(f,))
        S.dma("sp", dram, dap, f, f[0:64, 0:n])

    ntile = 0
    for g0 in range(0, T, 4):
        ng = min(4, T - g0)
        N = ng * 128
        c0 = g0 * 128
        S.dma("sp", ctab, ctab[:, 0:N], io["ct"], io["ct"].ap()[:, c0:c0 + N])
        S.dma("sp", stab, stab[:, 0:N], io["st"], io["st"].ap()[:, c0:c0 + N])
        for t4 in range(ng):
            x = hs[ntile % 2]
            ntile += 1
            S.dma("sp", x, x.ap(), io["h"], io["h"].ap()[(g0 + t4) * 128:(g0 + t4 + 1) * 128, :])
            emit_transpose_h(S, x, hT, t4, cst, cst.ap())
        for t4 in range(ng):
            r0 = (g0 + t4) * 128
            pa, pb = S.psum(), S.psum()
            for c in range(16):
                S.mm(pa, pa[:, 0:512], hT, hT[:, c, t4 * 128:(t4 + 1) * 128], win, win[:, c, 0:512], c == 0, c == 15)
            for c in range(16):
                S.mm(pb, pb[:, 0:256], hT, hT[:, c, t4 * 128:(t4 + 1) * 128], win, win[:, c, 512:768], c == 0, c == 15)
            S.op("act", lambda h_, pa=pa: h_.copy(out=lat[:, 0:512], in_=pa.ap()), reads=(pa,), writes=(lat,))
            S.op("act", lambda h_, pb=pb: h_.copy(out=lat[:, 512:768], in_=pb[:, 0:256]), reads=(pb,), writes=(lat,))
            for (lo, hi, col) in ((0, 512, 0), (512, 768, 1)):
                S.op("dve", lambda h_, lo=lo, hi=hi, col=col: h_.tensor_tensor(out=junk[:, 0:hi - lo], in0=lat[:, lo:hi],
                                                                               in1=lat[:, lo:hi], op=ALU.mult),
                     reads=(lat,), writes=(junk,))
                S.op("dve", lambda h_, lo=lo, hi=hi, col=col: h_.reduce_sum(out=rr[:, col:col + 1], in_=junk[:, 0:hi - lo],
                                                                            axis=AX.X), reads=(junk,), writes=(rr,))
                S.op("dve", lambda h_, lo=lo, hi=hi, col=col: h_.tensor_scalar(out=rr[:, col:col + 1], in0=rr[:, col:col + 1],
                                                                               scalar1=1.0 / (hi - lo), scalar2=RMS_EPS,
                                                                               op0=ALU.mult, op1=ALU.add),
                     reads=(rr,), writes=(rr,))
            S.op("act", lambda h_: h_.sqrt(out=rr[:, 0:2], in_=rr[:, 0:2]), reads=(rr,), writes=(rr,))
            S.op("dve", lambda h_: h_.reciprocal(out=rr[:, 0:2], in_=rr[:, 0:2]), reads=(rr,), writes=(rr,))
            S.op("dve", lambda h_: h_.tensor_scalar(out=latn[:, 0:512], in0=lat[:, 0:512], scalar1=rr[:, 0:1], scalar2=None,
                                                    op0=ALU.mult), reads=(lat, rr), writes=(latn,))
            S.op("dve", lambda h_: h_.tensor_scalar(out=latn[:, 512:768], in0=lat[:, 512:768], scalar1=rr[:, 1:2],
                                                    scalar2=None, op0=ALU.mult), reads=(lat, rr), writes=(latn,))
            ps = S.psum()
            psb = ps.ap().bitcast(BF16)
            for c in range(6):
                S.tr(ps, psb[:, c * 128:(c + 1) * 128], latn, latn[:, c * 128:(c + 1) * 128], identb, identb.ap())
            S.op("dve", lambda h_, psb=psb, t4=t4: h_.tensor_copy(out=latnT[:, :, t4 * 128:(t4 + 1) * 128],
                                                                  in_=psb[:, 0:768].rearrange("p (c t) -> p c t", t=128)),
                 reads=(ps,), writes=(latnT,))
            pv = S.psum()
            for c in range(16):
                S.mm(pv, pv.ap(), hT, hT[:, c, t4 * 128:(t4 + 1) * 128], win, win[:, c, 1856:2368], c == 0, c == 15)
            tt = to[nto[0] % 2]
            nto[0] += 1
            S.op("act", lambda h_, pv=pv, tt=tt: h_.copy(out=tt.ap(), in_=pv.ap()), reads=(pv,), writes=(tt,))
            S.dma("sp", io["sbv"], io["sbv"].ap()[r0:r0 + 128, :], tt, tt.ap())
        pa, pb = S.psum(), S.psum()
        for c in range(16):
            S.mm(pa, pa[0:64, 0:N], win, win[:, c, 768:832], hT, hT[:, c, 0:N], c == 0, c == 15)
        for c in range(16):
            S.mm(pb, pb[0:64, 0:N], winsw, winsw[:, c, :], hT, hT[:, c, 0:N], c == 0, c == 15)
        rope_out(pa, pb, N, io["krt"], io["krt"].ap()[:, c0:c0 + N])
        for (off, dst) in ((832, io["sbqt"]), (1344, io["sbkt"])):
            for fc in range(4):
                ps = S.psum()
                for c in range(16):
                    S.mm(ps, ps[:, 0:N], win, win[:, c, off + fc * 128: off + (fc + 1) * 128], hT, hT[:, c, 0:N],
                         c == 0, c == 15)
                fm_out(ps, 128, N, dst, dst.ap()[fc * 128:(fc + 1) * 128, c0:c0 + N])
        for hh in range(HA):
            ps = S.psum()
            for c in range(4):
                S.mm(ps, ps[:, 0:N], wuq, wuq[:, c, hh * 192: hh * 192 + 128], latnT, latnT[:, c, 0:N], c == 0, c == 3)
            fm_out(ps, 128, N, io["qat"], io["qat"].ap()[hh * 192: hh * 192 + 128, c0:c0 + N])
            pa, pb = S.psum(), S.psum()
            for c in range(4):
                S.mm(pa, pa[0:64, 0:N], wuq, wuq[:, c, hh * 192 + 128: hh * 192 + 192], latnT, latnT[:, c, 0:N], c == 0, c == 3)
            for c in range(4):
                S.mm(pb, pb[0:64, 0:N], wuqsw, wuqsw[:, c, hh * 64:(hh + 1) * 64], latnT, latnT[:, c, 0:N], c == 0, c == 3)
            rope_out(pa, pb, N, io["qat"], io["qat"].ap()[hh * 192 + 128: hh * 192 + 192, c0:c0 + N])
        for hh in range(HA):
            ps = S.psum()
            for c in range(2):
                S.mm(ps, ps[:, 0:N], wukv, wukv[:, c, hh * 256: hh * 256 + 128], latnT, latnT[:, 4 + c, 0:N], c == 0, c == 1)
            fm_out(ps, 128, N, io["knt"], io["knt"].ap()[hh * 128:(hh + 1) * 128, c0:c0 + N])
        for t4 in range(ng):
            r0 = (g0 + t4) * 128
            pv = S.psum()
            for c in range(2):
                S.mm(pv, pv.ap(), latnT, latnT[:, 4 + c, t4 * 128:(t4 + 1) * 128], wv_v, wv_v[:, c, :], c == 0, c == 1)
            tt = to[nto[0] % 2]
            nto[0] += 1
            S.op("dve", lambda h_, pv=pv, tt=tt: h_.tensor_copy(out=tt.ap(), in_=pv.ap()), reads=(pv,), writes=(tt,))
            S.dma("sp", io["va"], io["va"].ap()[r0:r0 + 128, :], tt, tt.ap())
    ph.__exit__(None, None, None)


def rope_tables(pos):
    half = A_ROPE // 2
    inv = (10000.0 ** (-np.arange(half, dtype=np.float32) / half)).astype(np.float32)
    ang = pos.astype(np.float32)[None, :] * inv[:, None]
    c, s = np.cos(ang).astype(np.float32), np.sin(ang).astype(np.float32)
    return np.concatenate([c, c], 0), np.concatenate([-s, s], 0)


def own_positions(r, T=TPC):
    j = np.arange(T)
    return (((2 * j + r) * 128)[:, None] + np.arange(128)[None, :]).reshape(-1)


EV_A_OUT = (("qat", [HA * 192, None]), ("sbqt", [512, None]), ("knt", [512, None]), ("krt", [64, None]),
            ("sbkt", [512, None]), ("va", [None, 512]), ("sbv", [None, 512]))


def build_ev_a(T):
    nc = bass.Bass("TRN2", target_bir_lowering=False)
    S = Sched(nc)
    S.init_psum()
    io = {}
    NTK = T * 128
    for name, shape in (("h", [NTK, D]), ("w_in", [D, EV_IN]), ("qn", [1, Q_LORA]), ("kvn", [1, KV_LORA]),
                        ("w_uq", [Q_LORA, 768]), ("w_ukv", [KV_LORA, 1024]), ("ct", [64, NTK]), ("st", [64, NTK]),
                        ("cst", [128, 416])):
        io[name] = S.dram(name, shape, F32, kind="ExternalInput")
    for name, shape in EV_A_OUT:
        io[name] = S.dram(name, [NTK if s is None else s for s in shape], BF16, kind="ExternalOutput")
    emit_ev_a(S, io, T)
    S.barrier(["sp"])
    return nc


MLA_SCALE = (A_NOPE + A_ROPE) ** -0.5
SB_SCALE = B_DH ** -0.5
NEG = -1.0e30


def ev_masks(r):
    i = np.arange(128)[:, None]
    kk = np.arange(384)[None, :]
    lim = 16 + 64 * ((128 * r + i - 16) // 64 + 1)
    mla = np.where(kk < lim, 0.0, NEG).astype(np.float32)
    s = np.arange(128)[:, None]
    q = np.arange(128)[None, :]
    tri = (s < q).astype(np.float32)
    m0 = tri if r == 0 else np.ones((128, 128), np.float32)
    m1 = np.zeros((128, 128), np.float32) if r == 0 else tri
    sbm = np.concatenate([m0, m1], 1)
    cst = np.zeros((128, 512), np.float32)
    cst[:, 0:128] = np.eye(128)
    cst[:, 128:256] = (np.arange(128)[:, None] >= np.arange(128)[None, :])
    cst[:, 256:384] = 1.0
    return mla, sbm, cst


def emit_outproj_ln(S, io, T, mergedT, nchunk, wname):
    ph = S.scope()
    ph.__enter__()
    wo = S.sbuf("wo", [128, nchunk, D], BF16)
    wd = io[wname]
    for c in range(nchunk):
        load_cast(S, wo, wo[:, c, :], wd, wd.ap()[c * 128:(c + 1) * 128, :])
    gbc = S.sbuf("gbcB", [128, D], F32)
    bbc = S.sbuf("bbcB", [128, D], F32)
    S.dma("sp", gbc, gbc.ap(), io["lng"], io["lng"].ap().partition_broadcast(128))
    S.dma("sp", bbc, bbc.ap(), io["lnb"], io["lnb"].ap().partition_broadcast(128))
    hs = [S.sbuf(f"hsB{i}", [128, D], F32) for i in range(2)]
    mt = [S.sbuf(f"mtB{i}", [128, nchunk, 128], BF16) for i in range(2)]
    zz = [S.sbuf(f"zzB{i}", [128, D], F32) for i in range(2)]
    oo = [S.sbuf(f"ooB{i}", [128, D], F32) for i in range(2)]
    st = S.sbuf("lnstB", [128, 64], F32)
    for i in range(T):
        x, m, z, o = hs[i % 2], mt[i % 2], zz[i % 2], oo[i % 2]
        S.dma("sp", x, x.ap(), io["h"], io["h"].ap()[i * 128:(i + 1) * 128, :])
        S.dma("sp", m, m.ap(), mergedT, mergedT.ap()[:, i * 128:(i + 1) * 128].rearrange("(c p) t -> p c t", p=128))
        for cg in range(4):
            ps = S.psum()
            for c in range(nchunk):
                S.mm(ps, ps.ap(), m, m[:, c, :], wo, wo[:, c, cg * 512:(cg + 1) * 512], c == 0, c == nchunk - 1)
            S.op("dve", lambda h_, ps=ps, cg=cg, x=x, z=z: h_.scalar_tensor_tensor(
                out=z[:, cg * 512:(cg + 1) * 512], in0=x[:, cg * 512:(cg + 1) * 512], scalar=float(DN_ALPHA), in1=ps.ap(),
                op0=ALU.mult, op1=ALU.add), reads=(ps, x), writes=(z,))
        rstd, nmr = _ln_tile(S, z, z, gbc, bbc, None, st, st)
        S.op("act", lambda h_, z=z, o=o: h_.activation(out=o.ap(), in_=z.ap(), func=AF.Identity, bias=nmr, scale=rstd),
             reads=(z, st), writes=(o,))
        S.op("pool", lambda h_, o=o: h_.tensor_tensor(out=o.ap(), in0=o.ap(), in1=gbc.ap(), op=ALU.mult),
             reads=(o, gbc), writes=(o,))
        S.op("pool", lambda h_, o=o: h_.tensor_tensor(out=o.ap(), in0=o.ap(), in1=bbc.ap(), op=ALU.add),
             reads=(o, bbc), writes=(o,))
        S.dma("sp", io["out"], io["out"].ap()[i * 128:(i + 1) * 128, :], o, o.ap())
    ph.__exit__(None, None, None)


def emit_softmax_pv(S, Srow, nk, scale, P, PT, V, identb, sm, osb):
    n = nk * 128
    MX, NM, RS_, RI = sm[:, 0:1], sm[:, 1:2], sm[:, 2:3], sm[:, 3:4]
    S.op("dve", lambda h_: h_.reduce_max(out=MX, in_=Srow[:, 0:n], axis=AX.X), reads=(Srow,), writes=(sm,))
    S.op("dve", lambda h_: h_.tensor_scalar(out=NM, in0=MX, scalar1=-float(scale), scalar2=None, op0=ALU.mult),
         reads=(sm,), writes=(sm,))
    S.op("act", lambda h_: h_.activation(out=P[:, 0:n], in_=Srow[:, 0:n], func=AF.Exp, bias=NM, scale=float(scale),
                                         accum_out=RS_), reads=(Srow, sm), writes=(P, sm))
    for g in range(0, nk, 8):
        m = min(8, nk - g)
        ps = S.psum()
        psb = ps.ap().bitcast(BF16)
        for i in range(m):
            S.tr(ps, psb[:, i * 128:(i + 1) * 128], P, P[:, (g + i) * 128:(g + i + 1) * 128], identb, identb.ap())
        src = psb[:, 0:m * 128].rearrange("p (c t) -> p c t", t=128)
        if (g // 8) % 2 == 0:
            S.op("dve", lambda h_, src=src, g=g, m=m: h_.tensor_copy(out=PT[:, g:g + m, :], in_=src), reads=(ps,), writes=(PT,))
        else:
            S.op("act", lambda h_, src=src, g=g, m=m: h_.copy(out=PT[:, g:g + m, :], in_=src), reads=(ps,), writes=(PT,))
    po = S.psum()
    for kt in range(nk):
        S.mm(po, po[:, 0:128], PT, PT[:, kt, :], V, V[:, kt, :], kt == 0, kt == nk - 1)
    S.op("dve", lambda h_: h_.reciprocal(out=RI, in_=RS_), reads=(sm,), writes=(sm,))
    S.op("dve", lambda h_: h_.tensor_scalar(out=osb.ap(), in0=po[:, 0:128], scalar1=RI, scalar2=None, op0=ALU.mult),
         reads=(po, sm), writes=(osb,))


def emit_ev_b(S, io, T):
    nc = S.nc
    NTK = T * 128
    NKT = 2 * T + 1
    NKA = NKT * 128
    mergedT = S.dram("mergedT", [1024, NTK], BF16)
    ph = S.scope()
    ph.__enter__()
    cst = S.sbuf("cstB", [128, 512], F32)
    S.dma("sp", cst, cst.ap(), io["cstb"], io["cstb"].ap())
    identb = S.sbuf("identbB", [128, 128], BF16)
    tribI = S.sbuf("tribI", [128, 128], BF16)
    onesb = S.sbuf("onesbB", [128, 128], BF16)
    zerob = S.sbuf("zerobB", [128, 128], BF16)
    for (dst, lo) in ((identb, 0), (tribI, 128), (onesb, 256), (zerob, 384)):
        S.op("dve", lambda h_, dst=dst, lo=lo: h_.tensor_copy(out=dst.ap(), in_=cst[:, lo:lo + 128]), reads=(cst,), writes=(dst,))
    mmask = S.sbuf("mmask", [128, 384], F32)
    S.dma("sp", mmask, mmask.ap(), io["mla_mask"], io["mla_mask"].ap())
    sbm = S.sbuf("sbm", [128, 256], F32)
    S.dma("sp", sbm, sbm.ap(), io["sb_mask"], io["sb_mask"].ap())

    ph2 = S.scope()
    ph2.__enter__()
    KN = S.sbuf("KN", [128, NKA], BF16)
    KR = S.sbuf("KR", [64, NKA], BF16)
    V = S.sbuf("Vm", [128, NKT, 128], BF16)
    QN = S.sbuf("QN", [128, NTK], BF16)
    QR = S.sbuf("QR", [64, NTK], BF16)
    Srow = S.sbuf("Srow", [128, NKA], F32)
    P = S.sbuf("Pm", [128, NKA], BF16)
    PT = S.sbuf("PTm", [128, NKT, 128], BF16)
    sm = S.sbuf("smB", [128, 8], F32)
    osb = S.sbuf("osb", [128, 128], BF16)
    ot = [S.sbuf(f"otB{i}", [128, 128], BF16) for i in range(2)]
    S.dma("sp", KR, KR.ap(), io["krt"], io["krt"].ap())
    for hh in range(HA):
        S.dma("sp", KN, KN.ap(), io["knt"], io["knt"].ap()[hh * 128:(hh + 1) * 128, :])
        S.dma("sp", V, V.ap(), io["va"], io["va"].ap()[:, hh * 128:(hh + 1) * 128].rearrange("(t p) d -> p t d", p=128))
        S.dma("sp", QN, QN.ap(), io["qat"], io["qat"].ap()[hh * 192: hh * 192 + 128, :])
        S.dma("sp", QR, QR.ap(), io["qat"], io["qat"].ap()[hh * 192 + 128: hh * 192 + 192, :])
        for j in range(T):
            nk = 2 * j + 3
            n = nk * 128
            for g in range(0, n, 512):
                w = min(512, n - g)
                ps = S.psum()
                S.mm(ps, ps[:, 0:w], QN, QN[:, j * 128:(j + 1) * 128], KN, KN[:, g:g + w], True, False)
                S.mm(ps, ps[:, 0:w], QR, QR[:, j * 128:(j + 1) * 128], KR, KR[:, g:g + w], False, True)
                if (g // 512) % 2 == 0:
                    S.op("act", lambda h_, ps=ps, g=g, w=w: h_.copy(out=Srow[:, g:g + w], in_=ps[:, 0:w]), reads=(ps,), writes=(Srow,))
                else:
                    S.op("dve", lambda h_, ps=ps, g=g, w=w: h_.tensor_copy(out=Srow[:, g:g + w], in_=ps[:, 0:w]), reads=(ps,), writes=(Srow,))
            S.op("dve", lambda h_, n=n: h_.tensor_tensor(out=Srow[:, n - 384:n], in0=Srow[:, n - 384:n], in1=mmask.ap(), op=ALU.add),
                 reads=(Srow, mmask), writes=(Srow,))
            emit_softmax_pv(S, Srow, nk, MLA_SCALE, P, PT, V, identb, sm, osb)
            ps = S.psum()
            psb = ps.ap().bitcast(BF16)
            S.tr(ps, psb[:, 0:128], osb, osb.ap(), identb, identb.ap())
            o = ot[j % 2]
            S.op("act", lambda h_, psb=psb, o=o: h_.copy(out=o.ap(), in_=psb[:, 0:128]), reads=(ps,), writes=(o,))
            S.dma("sp", mergedT, mergedT.ap()[hh * 128:(hh + 1) * 128, j * 128:(j + 1) * 128], o, o.ap())
    ph2.__exit__(None, None, None)

    ph2 = S.scope()
    ph2.__enter__()
    KT = S.sbuf("KTs", [128, NKA], BF16)
    V = S.sbuf("Vs", [128, NKT, 128], BF16)
    QT = S.sbuf("QTs", [128, NTK], BF16)
    E = [S.sbuf(f"Es{i}", [128, 512], F32) for i in range(2)]
    SPB = [S.sbuf(f"SPB{i}", [128, 512], BF16) for i in range(2)]
    EX = [S.sbuf(f"EXs{i}", [128, 512], F32) for i in range(2)]
    AT = [S.sbuf(f"ATs{i}", [128, 512], BF16) for i in range(2)]
    SPS = S.sbuf("SPS", [128, 512], F32)
    SPSB = [S.sbuf(f"SPSB{i}", [128, 512], BF16) for i in range(2)]
    OS = [S.sbuf(f"OSs{i}", [128, 512], BF16) for i in range(2)]
    step = 0
    for hh in range(HB):
        S.dma("sp", KT, KT.ap(), io["sbkt"], io["sbkt"].ap()[hh * 128:(hh + 1) * 128, :])
        S.dma("sp", V, V.ap(), io["sbv"], io["sbv"].ap()[:, hh * 128:(hh + 1) * 128].rearrange("(t p) d -> p t d", p=128))
        S.dma("sp", QT, QT.ap(), io["sbqt"], io["sbqt"].ap()[hh * 128:(hh + 1) * 128, :])
        for j0 in range(0, T, 4):
            ng = min(4, T - j0)
            N = ng * 128
            q0 = j0 * 128
            S.op("pool", lambda h_: h_.memset(SPS.ap(), 0.0), writes=(SPS,))
            S.op("pool", lambda h_: h_.memset(SPSB[step % 2].ap(), 0.0), writes=(SPSB[step % 2],))
            po = S.psum_hold()
            S.mm(po, po[:, 0:N], zerob, zerob.ap(), QT, QT[:, q0:q0 + N], True, False)
            kt_max = 2 * (j0 + ng - 1) + 1
            for kt in range(kt_max, -1, -1):
                jj_min = max(j0, kt // 2)
                a0 = (jj_min - j0) * 128
                e_, spb, ex, at = E[step % 2], SPB[step % 2], EX[step % 2], AT[step % 2]
                spsb_prev, spsb_next = SPSB[step % 2], SPSB[(step + 1) % 2]
                step += 1
                pz = S.psum()
                S.mm(pz, pz[:, a0:N], KT, KT[:, kt * 128:(kt + 1) * 128], QT, QT[:, q0 + a0:q0 + N], True, True)
                S.op("act", lambda h_, pz=pz, e_=e_, a0=a0, N=N: h_.activation(out=e_[:, a0:N], in_=pz[:, a0:N], func=AF.Exp,
                                                                              scale=float(SB_SCALE)), reads=(pz,), writes=(e_,))
                for jj in range(jj_min, j0 + ng):
                    d = kt - 2 * jj
                    if d in (0, 1):
                        b0 = (jj - j0) * 128
                        S.op("dve", lambda h_, e_=e_, b0=b0, d=d: h_.tensor_tensor(out=e_[:, b0:b0 + 128], in0=e_[:, b0:b0 + 128],
                                                                                   in1=sbm[:, d * 128:(d + 1) * 128], op=ALU.mult),
                             reads=(e_, sbm), writes=(e_,))
                S.op("act", lambda h_, e_=e_, spb=spb, a0=a0, N=N: h_.activation(out=spb[:, a0:N], in_=e_[:, a0:N], func=AF.Ln,
                                                                                bias=1.0, scale=1.0), reads=(e_,), writes=(spb,))
                pc = S.psum()
                S.mm(pc, pc[:, a0:N], tribI, tribI.ap(), spb, spb[:, a0:N], True, False)
                S.mm(pc, pc[:, a0:N], onesb, onesb.ap(), spsb_prev, spsb_prev[:, a0:N], False, True)
                S.op("act", lambda h_, pc=pc, ex=ex, a0=a0, N=N: h_.activation(out=ex[:, a0:N], in_=pc[:, a0:N], func=AF.Exp,
                                                                              scale=-1.0), reads=(pc,), writes=(ex,))
                S.op("dve", lambda h_, e_=e_, ex=ex, at=at, a0=a0, N=N: h_.tensor_tensor(out=at[:, a0:N], in0=e_[:, a0:N],
                                                                                        in1=ex[:, a0:N], op=ALU.mult),
                     reads=(e_, ex), writes=(at,))
                S.mm(po, po[:, a0:N], V, V[:, kt, :], at, at[:, a0:N], False, kt == 0)
                if kt > 0:
                    S.op("pool", lambda h_, spb=spb, a0=a0, N=N: h_.tensor_tensor(out=SPS[:, a0:N], in0=SPS[:, a0:N],
                                                                                 in1=spb[:, a0:N], op=ALU.add),
                         reads=(SPS, spb), writes=(SPS,))
                    S.op("pool", lambda h_, spsb_next=spsb_next: h_.tensor_copy(out=spsb_next.ap(), in_=SPS.ap()),
                         reads=(SPS,), writes=(spsb_next,))
            o = OS[(j0 // 4) % 2]
            S.op("act", lambda h_, po=po, o=o, N=N: h_.copy(out=o[:, 0:N], in_=po[:, 0:N]), reads=(po,), writes=(o,))
            S.psum_release(po)
            S.dma("sp", mergedT, mergedT.ap()[(HA + hh) * 128:(HA + hh + 1) * 128, q0:q0 + N], o, o[:, 0:N])
    ph2.__exit__(None, None, None)
    ph.__exit__(None, None, None)
    emit_outproj_ln(S, io, T, mergedT, 8, "w_out")


def build_ev_b(T):
    nc = bass.Bass("TRN2", target_bir_lowering=False)
    S = Sched(nc)
    S.init_psum()
    io = {}
    NTK = T * 128
    NKA = (2 * T + 1) * 128
    for name, shape in (("qat", [768, NTK]), ("sbqt", [512, NTK]), ("knt", [512, NKA]), ("krt", [64, NKA]),
                        ("sbkt", [512, NKA]), ("va", [NKA, 512]), ("sbv", [NKA, 512])):
        io[name] = S.dram(name, shape, BF16, kind="ExternalInput")
    for name, shape in (("h", [NTK, D]), ("w_out", [1024, D]), ("lng", [1, D]), ("lnb", [1, D]),
                        ("mla_mask", [128, 384]), ("sb_mask", [128, 256]), ("cstb", [128, 512])):
        io[name] = S.dram(name, shape, F32, kind="ExternalInput")
    io["out"] = S.dram("out", [NTK, D], F32, kind="ExternalOutput")
    emit_ev_b(S, io, T)
    S.barrier(["sp"])
    return nc


def interleave(parts, axis, T, extra_tiles=1):
    a, b = parts
    sh = list(a.shape)
    sh[axis] = (2 * T + extra_tiles) * 128
    out = np.zeros(sh, a.dtype)
    ov = np.moveaxis(out, axis, 0)
    av = np.moveaxis(a, axis, 0)
    bv = np.moveaxis(b, axis, 0)
    ov = ov.reshape((2 * T + extra_tiles, 128) + ov.shape[1:])
    ov[0:2 * T:2] = av.reshape((T, 128) + av.shape[1:])
    ov[1:2 * T:2] = bv.reshape((T, 128) + bv.shape[1:])
    return out


HC, C_DH, IDX_H, IDX_DH, TOPK = 8, 128, 8, 64, 256
OD_IN = 1864
C_SCALE = C_DH ** -0.5
N_BISECT = 22


def t5_bucket_np(rel):
    rel = np.asarray(rel)
    half, me = 16, 8
    base = np.where(rel > 0, half, 0)
    n = np.abs(rel)
    nf = np.maximum(n, 1).astype(np.float32)
    large = me + (np.log(nf / np.float32(me)) / np.float32(np.log(128 / 8)) * np.float32(half - me)).astype(np.int32)
    large = np.minimum(large, half - 1)
    return base + np.where(n < me, n, large)


def od_consts():
    b = t5_bucket_np(np.arange(768) - 383)
    oh = np.zeros((32, 768), np.float32)
    oh[b, np.arange(768)] = 1.0
    oh[15, :] -= 1.0
    pw2 = np.tile((0.5 ** np.arange(32, dtype=np.float64)).astype(np.float32)[None, :], (128, 1))
    return oh, pw2


def emit_od_a(S, io, T):
    ph = S.scope()
    ph.__enter__()
    cst = S.sbuf("cstA", [128, 128], F32)
    S.dma("sp", cst, cst.ap(), io["cst"], io["cst"].ap()[:, 0:128])
    win = S.sbuf("winO", [128, 16, OD_IN], BF16)
    wd = io["w_in"]
    for c in range(16):
        load_cast(S, win, win[:, c, 0:1024], wd, wd.ap()[c * 128:(c + 1) * 128, 0:1024])
        load_cast(S, win, win[:, c, 1024:OD_IN], wd, wd.ap()[c * 128:(c + 1) * 128, 1024:OD_IN])
    hs = [S.sbuf(f"hsA{i}", [128, D], F32) for i in range(2)]
    hT = S.sbuf("hTA", [128, 16, 512], BF16)
    fo = [S.sbuf(f"foA{i}", [128, 512], BF16) for i in range(3)]
    to = [S.sbuf(f"toA{i}", [128, 128], BF16) for i in range(2)]
    tw = [S.sbuf(f"twA{i}", [128, 8], F32) for i in range(2)]
    nfo = [0]

    def fm_out(ps, rows, n, dram, dap):
        f = fo[nfo[0] % 3]
        nfo[0] += 1
        if nfo[0] % 2:
            S.op("dve", lambda h_: h_.tensor_copy(out=f[0:rows, 0:n], in_=ps[0:rows, 0:n]), reads=(ps,), writes=(f,))
        else:
            S.op("act", lambda h_: h_.copy(out=f[0:rows, 0:n], in_=ps[0:rows, 0:n]), reads=(ps,), writes=(f,))
        S.dma("sp", dram, dap, f, f[0:rows, 0:n])

    ntile = 0
    for g0 in range(0, T, 4):
        ng = min(4, T - g0)
        N = ng * 128
        c0 = g0 * 128
        for t4 in range(ng):
            x = hs[ntile % 2]
            ntile += 1
            S.dma("sp", x, x.ap(), io["h"], io["h"].ap()[(g0 + t4) * 128:(g0 + t4 + 1) * 128, :])
            emit_transpose_h(S, x, hT, t4, cst, cst.ap())
        for (off, rows, dst, r0) in ([(fc * 128, 128, io["cqt"], fc * 128) for fc in range(8)] + [(1024, 128, io["ckt"], 0)] +
                                     [(1280 + fc * 128, 128, io["iqt"], fc * 128) for fc in range(4)] + [(1792, 64, io["ikt"], 0)]):
            ps = S.psum()
            for c in range(16):
                S.mm(ps, ps[0:rows, 0:N], win, win[:, c, off:off + rows], hT, hT[:, c, 0:N], c == 0, c == 15)
            fm_out(ps, rows, N, dst, dst.ap()[r0:r0 + rows, c0:c0 + N])
        for t4 in range(ng):
            r0 = (g0 + t4) * 128
            pv = S.psum()
            for c in range(16):
                S.mm(pv, pv[:, 0:128], hT, hT[:, c, t4 * 128:(t4 + 1) * 128], win, win[:, c, 1152:1280], c == 0, c == 15)
            for c in range(16):
                S.mm(pv, pv[:, 128:136], hT, hT[:, c, t4 * 128:(t4 + 1) * 128], win, win[:, c, 1856:1864], c == 0, c == 15)
            tt, ww = to[t4 % 2], tw[t4 % 2]
            S.op("act", lambda h_, pv=pv, tt=tt: h_.copy(out=tt.ap(), in_=pv[:, 0:128]), reads=(pv,), writes=(tt,))
            S.op("act", lambda h_, pv=pv, ww=ww: h_.copy(out=ww.ap(), in_=pv[:, 128:136]), reads=(pv,), writes=(ww,))
            S.dma("sp", io["cv"], io["cv"].ap()[r0:r0 + 128, :], tt, tt.ap())
            S.dma("sp", io["iw"], io["iw"].ap()[r0:r0 + 128, :], ww, ww.ap())
    ph.__exit__(None, None, None)


OD_A_OUT = (("cqt", [1024, None], BF16), ("ckt", [128, None], BF16), ("cv", [None, 128], BF16), ("iqt", [512, None], BF16),
            ("ikt", [64, None], BF16), ("iw", [None, 8], F32))


def build_od_a(T):
    nc = bass.Bass("TRN2", target_bir_lowering=False)
    S = Sched(nc)
    S.init_psum()
    io = {}
    NTK = T * 128
    for name, shape in (("h", [NTK, D]), ("w_in", [D, OD_IN]), ("cst", [128, 416])):
        io[name] = S.dram(name, shape, F32, kind="ExternalInput")
    for name, shape, dt in OD_A_OUT:
        io[name] = S.dram(name, [NTK if s is None else s for s in shape], dt, kind="ExternalOutput")
    emit_od_a(S, io, T)
    S.barrier(["sp"])
    return nc


def emit_od_b(S, io, T):
    nc = S.nc
    NTK = T * 128
    NKT = 2 * T + 1
    NKA = NKT * 128
    mergedT = S.dram("mergedTo", [1024, NTK], BF16)
    gdram = S.dram("gdram", [8, 768], F32)
    ph = S.scope()
    ph.__enter__()
    cst = S.sbuf("cstB", [128, 512], F32)
    S.dma("sp", cst, cst.ap(), io["cstb"], io["cstb"].ap())
    identb = S.sbuf("identbB", [128, 128], BF16)
    S.op("dve", lambda h_: h_.tensor_copy(out=identb.ap(), in_=cst[:, 0:128]), reads=(cst,), writes=(identb,))
    mmask = S.sbuf("mmask", [128, 384], F32)
    S.dma("sp", mmask, mmask.ap(), io["mla_mask"], io["mla_mask"].ap())
    pw2 = S.sbuf("pw2", [128, 32], F32)
    S.dma("sp", pw2, pw2.ap(), io["pw2"], io["pw2"].ap())
    relt = S.sbuf("relt", [32, 8], F32)
    ohd = S.sbuf("ohd", [32, 768], F32)
    S.dma("sp", relt, relt.ap(), io["relt"], io["relt"].ap())
    S.dma("sp", ohd, ohd.ap(), io["ohd"], io["ohd"].ap())
    gsb = S.sbuf("gsb", [8, 768], F32)
    for q in range(2):
        ps = S.psum()
        S.mm(ps, ps[0:8, 0:384], relt, relt.ap(), ohd, ohd[:, q * 384:(q + 1) * 384], True, True)
        S.op("dve", lambda h_, ps=ps, q=q: h_.tensor_copy(out=gsb[:, q * 384:(q + 1) * 384], in_=ps[0:8, 0:384]),
             reads=(ps,), writes=(gsb,))
    S.dma("sp", gdram, gdram.ap(), gsb, gsb.ap())
    bias = S.sbuf("biasw", [128, 8, 512], F32)
    bias1 = S.sbuf("biasw1", [128, 8, 512], F32)
    for i in range(128):
        S.dma("sp", bias, bias[i:i + 1, :, :], gdram, gdram.ap()[:, 255 - i: 255 - i + 512])
    for i in range(128):
        if 127 - i >= 0:
            S.dma("sp", bias1, bias1[i:i + 1, :, :], gdram, gdram.ap()[:, 127 - i: 127 - i + 512])
    rsel = S.sbuf("rsel", [128, 2], F32)
    S.dma("sp", rsel, rsel.ap(), io["rsel"], io["rsel"].ap())
    for hh in range(8):
        S.op("dve", lambda h_, hh=hh: h_.tensor_scalar(out=bias[:, hh, :], in0=bias[:, hh, :], scalar1=rsel[:, 0:1], scalar2=None,
                                                       op0=ALU.mult), reads=(bias, rsel), writes=(bias,))
        S.op("dve", lambda h_, hh=hh: h_.scalar_tensor_tensor(out=bias[:, hh, :], in0=bias1[:, hh, :], scalar=rsel[:, 1:2],
                                                              in1=bias[:, hh, :], op0=ALU.mult, op1=ALU.add),
             reads=(bias, bias1, rsel), writes=(bias,))

    CK = S.sbuf("CK", [128, NKA], BF16)
    IK = S.sbuf("IK", [64, NKA], BF16)
    V = S.sbuf("Vc", [128, NKT, 128], BF16)
    S.dma("sp", CK, CK.ap(), io["ckt"], io["ckt"].ap())
    S.dma("sp", IK, IK.ap(), io["ikt"], io["ikt"].ap())
    S.dma("sp", V, V.ap(), io["cv"], io["cv"].ap().rearrange("(t p) d -> p t d", p=128))
    CQ = [S.sbuf(f"CQ{i}", [128, 8, 128], BF16) for i in range(2)]
    IQ = [S.sbuf(f"IQ{i}", [64, 8, 128], BF16) for i in range(2)]
    IW = [S.sbuf(f"IW{i}", [128, 8], F32) for i in range(2)]
    Srow = S.sbuf("SrowO", [128, NKA], F32)
    SMm = S.sbuf("SMm", [128, NKA], BF16)
    P = S.sbuf("Po", [128, NKA], BF16)
    PT = S.sbuf("PTo", [128, NKT, 128], BF16)
    rl = [S.sbuf(f"rl{i}", [128, 512], F32) for i in range(2)]
    sm = S.sbuf("smO", [128, 8], F32)
    bs = S.sbuf("bsO", [128, 64], F32)
    osb = S.sbuf("osbO", [128, 128], BF16)
    ot = [S.sbuf(f"otO{i}", [128, 128], BF16) for i in range(2)]
    BB, LO, MID, CNT, TT = bs[:, 0:1], bs[:, 1:2], bs[:, 2:3], bs[:, 3:4], bs[:, 4:5]
    HK = bs[:, 32:64]
    nrl = 0
    for j in range(T):
        nk = 2 * j + 3
        n = nk * 128
        cq, iq, iw = CQ[j % 2], IQ[j % 2], IW[j % 2]
        S.dma("sp", cq, cq.ap(), io["cqt"], io["cqt"].ap()[:, j * 128:(j + 1) * 128].rearrange("(h p) t -> p h t", p=128))
        S.dma("sp", iq, iq.ap(), io["iqt"], io["iqt"].ap()[:, j * 128:(j + 1) * 128].rearrange("(h p) t -> p h t", p=64))
        S.dma("sp", iw, iw.ap(), io["iw"], io["iw"].ap()[j * 128:(j + 1) * 128, :])
        for g in range(0, n, 512):
            w = min(512, n - g)
            for hh in range(8):
                ps = S.psum()
                S.mm(ps, ps[:, 0:w], iq, iq[:, hh, :], IK, IK[:, g:g + w], True, True)
                r_ = rl[nrl % 2]
                nrl += 1
                S.op("act", lambda h_, ps=ps, r_=r_, w=w: h_.activation(out=r_[:, 0:w], in_=ps[:, 0:w], func=AF.Relu),
                     reads=(ps,), writes=(r_,))
                if hh == 0:
                    S.op("dve", lambda h_, r_=r_, g=g, w=w, iw=iw: h_.tensor_scalar(out=Srow[:, g:g + w], in0=r_[:, 0:w],
                                                                                   scalar1=iw[:, 0:1], scalar2=None, op0=ALU.mult),
                         reads=(r_, iw), writes=(Srow,))
                else:
                    S.op("dve", lambda h_, r_=r_, g=g, w=w, iw=iw, hh=hh: h_.scalar_tensor_tensor(
                        out=Srow[:, g:g + w], in0=r_[:, 0:w], scalar=iw[:, hh:hh + 1], in1=Srow[:, g:g + w],
                        op0=ALU.mult, op1=ALU.add), reads=(r_, iw, Srow), writes=(Srow,))
        S.op("dve", lambda h_, n=n: h_.tensor_reduce(out=BB, in_=Srow[:, 0:n], axis=AX.X, op=ALU.max),
             reads=(Srow,), writes=(bs,))
        S.op("dve", lambda h_, n=n: h_.tensor_reduce(out=LO, in_=Srow[:, 0:n], axis=AX.X, op=ALU.min),
             reads=(Srow,), writes=(bs,))
        S.op("dve", lambda h_, n=n: h_.tensor_tensor(out=Srow[:, n - 384:n], in0=Srow[:, n - 384:n], in1=mmask.ap(), op=ALU.add),
             reads=(Srow, mmask), writes=(Srow,))
        S.op("dve", lambda h_: h_.tensor_tensor(out=BB, in0=BB, in1=LO, op=ALU.subtract), reads=(bs,), writes=(bs,))
        S.op("dve", lambda h_: h_.tensor_scalar(out=BB, in0=BB, scalar1=0.5005, scalar2=1e-20, op0=ALU.mult, op1=ALU.add),
             reads=(bs,), writes=(bs,))
        S.op("dve", lambda h_: h_.tensor_scalar(out=HK, in0=pw2.ap(), scalar1=BB, scalar2=None, op0=ALU.mult),
             reads=(bs, pw2), writes=(bs,))
        for k in range(N_BISECT):
            S.op("dve", lambda h_, k=k: h_.tensor_tensor(out=MID, in0=LO, in1=HK[:, k:k + 1], op=ALU.add), reads=(bs,), writes=(bs,))
            S.op("dve", lambda h_, n=n: h_.tensor_scalar(out=P[:, 0:n], in0=Srow[:, 0:n], scalar1=MID, scalar2=None,
                                                         op0=ALU.is_ge, op1=ALU.add, accum_out=CNT), reads=(Srow, bs), writes=(P, bs))
            S.op("dve", lambda h_, k=k: h_.scalar_tensor_tensor(out=TT, in0=CNT, scalar=float(TOPK) - 0.5, in1=HK[:, k:k + 1],
                                                                op0=ALU.is_ge, op1=ALU.mult), reads=(bs,), writes=(bs,))
            S.op("dve", lambda h_: h_.tensor_tensor(out=LO, in0=LO, in1=TT, op=ALU.add), reads=(bs,), writes=(bs,))
        S.op("dve", lambda h_, n=n: h_.tensor_scalar(out=SMm[:, 0:n], in0=Srow[:, 0:n], scalar1=LO, scalar2=1.0,
                                                     op0=ALU.is_ge, op1=ALU.subtract), reads=(Srow, bs), writes=(SMm,))
        for hh in range(HC):
            for g in range(0, n, 512):
                w = min(512, n - g)
                ps = S.psum()
                S.mm(ps, ps[:, 0:w], cq, cq[:, hh, :], CK, CK[:, g:g + w], True, True)
                S.op("dve", lambda h_, ps=ps, g=g, w=w: h_.scalar_tensor_tensor(
                    out=Srow[:, g:g + w], in0=SMm[:, g:g + w], scalar=1.0e30, in1=ps[:, 0:w], op0=ALU.mult, op1=ALU.add),
                    reads=(SMm, ps), writes=(Srow,))
            lo_k = 256 * j - 128
            b0 = 0
            if lo_k < 0:
                b0, lo_k = -lo_k, 0
            S.op("dve", lambda h_, hh=hh, lo_k=lo_k, b0=b0, n=n: h_.scalar_tensor_tensor(
                out=Srow[:, lo_k:n], in0=bias[:, hh, b0:512], scalar=float(1.0 / C_SCALE), in1=Srow[:, lo_k:n],
                op0=ALU.mult, op1=ALU.add), reads=(bias, Srow), writes=(Srow,))
            emit_softmax_pv(S, Srow, nk, C_SCALE, P, PT, V, identb, sm, osb)
            ps = S.psum()
            psb = ps.ap().bitcast(BF16)
            S.tr(ps, psb[:, 0:128], osb, osb.ap(), identb, identb.ap())
            o = ot[hh % 2]
            S.op("act", lambda h_, psb=psb, o=o: h_.copy(out=o.ap(), in_=psb[:, 0:128]), reads=(ps,), writes=(o,))
            S.dma("sp", mergedT, mergedT.ap()[hh * 128:(hh + 1) * 128, j * 128:(j + 1) * 128], o, o.ap())
    ph.__exit__(None, None, None)
    emit_outproj_ln(S, io, T, mergedT, 8, "w_out")


def build_od_b(T):
    nc = bass.Bass("TRN2", target_bir_lowering=False)
    S = Sched(nc)
    S.init_psum()
    io = {}
    NTK = T * 128
    NKA = (2 * T + 1) * 128
    for name, shape in (("cqt", [1024, NTK]), ("iqt", [512, NTK]), ("ckt", [128, NKA]), ("ikt", [64, NKA]), ("cv", [NKA, 128])):
        io[name] = S.dram(name, shape, BF16, kind="ExternalInput")
    for name, shape in (("iw", [NTK, 8]), ("h", [NTK, D]), ("w_out", [1024, D]), ("lng", [1, D]), ("lnb", [1, D]),
                        ("mla_mask", [128, 384]), ("cstb", [128, 512]), ("relt", [32, 8]), ("ohd", [32, 768]),
                        ("pw2", [128, 32]), ("rsel", [128, 2])):
        io[name] = S.dram(name, shape, F32, kind="ExternalInput")
    io["out"] = S.dram("out", [NTK, D], F32, kind="ExternalOutput")
    emit_od_b(S, io, T)
    S.barrier(["sp"])
    return nc


_PROGS = {}


def _prog(name, fn, T):
    key = (name, T)
    if key not in _PROGS:
        _PROGS[key] = fn(T)
    return _PROGS[key]


def _c(a, dt=np.float32):
    return np.ascontiguousarray(a, dtype=dt)


def _launch(nc, maps):
    res = run_bass_kernel_spmd(nc, maps, core_ids=list(range(len(maps))))
    return res.results


def kernel(x, meta_tokens, rel_bias_table, ev_w_in, mla_q_norm, mla_kv_norm, mla_w_uq, mla_w_ukv, ev_w_out,
           od_w_in, od_w_out, ln_g, ln_b, router_w, router_b, exp_w_in, exp_b_in, exp_w_out, exp_b_out):
    T = TPC
    B = x.shape[0]
    ncores = 2 * B
    x = np.asarray(x, np.float32)
    pos = [own_positions(r, T) for r in (0, 1)]
    h = []
    for c in range(ncores):
        b, r = divmod(c, 2)
        p = pos[r]
        hc = np.zeros((T * 128, D), np.float32)
        m_meta = p < N_META
        hc[m_meta] = np.asarray(meta_tokens, np.float32)[p[m_meta]]
        m_real = (p >= N_META) & (p < N_META + SEQ)
        hc[m_real] = x[b, p[m_real] - N_META]
        h.append(hc)
    cstm = moe_consts()
    ohd, pw2 = od_consts()
    rope = [rope_tables(pos[r]) for r in (0, 1)]
    masks = [ev_masks(r) for r in (0, 1)]
    rsel = []
    for r in (0, 1):
        s = np.zeros((128, 2), np.float32)
        s[:, r] = 1.0
        rsel.append(s)

    for i in range(DEPTH):
        j = i // 2
        if i % 2 == 0:
            ncA = _prog("ev_a", build_ev_a, T)
            mapsA = [dict(h=h[c], w_in=_c(ev_w_in[j]), qn=_c(mla_q_norm[j][None]), kvn=_c(mla_kv_norm[j][None]),
                          w_uq=_c(mla_w_uq[j]), w_ukv=_c(mla_w_ukv[j]), ct=rope[c % 2][0], st=rope[c % 2][1], cst=cstm)
                     for c in range(ncores)]
            RA = _launch(ncA, mapsA)
            ncB = _prog("ev_b", build_ev_b, T)
            mapsB = []
            for c in range(ncores):
                b, r = divmod(c, 2)
                m = {}
                for name, ax in (("knt", 1), ("krt", 1), ("sbkt", 1), ("va", 0), ("sbv", 0)):
                    if r == 0:
                        m[name] = interleave([RA[2 * b][name], RA[2 * b + 1][name]], ax, T)
                    else:
                        m[name] = mapsB[c - 1][name]
                m["qat"], m["sbqt"] = RA[c]["qat"], RA[c]["sbqt"]
                m.update(h=h[c], w_out=_c(ev_w_out[j]), lng=_c(ln_g[i, 0][None]), lnb=_c(ln_b[i, 0][None]),
                         mla_mask=masks[r][0], sb_mask=masks[r][1], cstb=masks[r][2])
                mapsB.append(m)
            RB = _launch(ncB, mapsB)
        else:
            ncA = _prog("od_a", build_od_a, T)
            mapsA = [dict(h=h[c], w_in=_c(od_w_in[j]), cst=cstm) for c in range(ncores)]
            RA = _launch(ncA, mapsA)
            ncB = _prog("od_b", build_od_b, T)
            mapsB = []
            for c in range(ncores):
                b, r = divmod(c, 2)
                m = {}
                for name, ax in (("ckt", 1), ("ikt", 1), ("cv", 0)):
                    if r == 0:
                        m[name] = interleave([RA[2 * b][name], RA[2 * b + 1][name]], ax, T)
                    else:
                        m[name] = mapsB[c - 1][name]
                m["cqt"], m["iqt"], m["iw"] = RA[c]["cqt"], RA[c]["iqt"], RA[c]["iw"]
                m.update(h=h[c], w_out=_c(od_w_out[j]), lng=_c(ln_g[i, 0][None]), lnb=_c(ln_b[i, 0][None]),
                         mla_mask=masks[r][0], cstb=masks[r][2], relt=_c(rel_bias_table), ohd=ohd, pw2=pw2, rsel=rsel[r])
                mapsB.append(m)
            RB = _launch(ncB, mapsB)
        ncM = _prog("moe", build_moe, T)
        w1, b1, w2, b2 = _c(exp_w_in[i]), _c(exp_b_in[i]), _c(exp_w_out[i]), _c(exp_b_out[i])
        rw, rb = _c(router_w[i]), _c(router_b[i][None])
        lg, lb = _c(ln_g[i, 1][None]), _c(ln_b[i, 1][None])
        mapsM = [dict(h=RB[c]["out"], rw=rw, rb=rb, w1=w1, b1=b1, w2=w2, b2=b2, lng=lg, lnb=lb, cst=cstm)
                 for c in range(ncores)]
        RM = _launch(ncM, mapsM)
        h = [RM[c]["out"] for c in range(ncores)]

    out = np.zeros((B, SEQ, D), np.float32)
    for c in range(ncores):
        b, r = divmod(c, 2)
        p = pos[r]
        m_real = (p >= N_META) & (p < N_META + SEQ)
        out[b, p[m_real] - N_META] = h[c][m_real]
    return out
```
